# Optimizing a Trainium2 kernel written in Bass

```python
import jax, jax.numpy as jnp
from jax import lax
import numpy as np

D_MODEL = 1024
BATCH = 32
SEQ = 2048
DEPTH = 1

HG_HEADS = 8
HG_DK = 128
HG_DV = D_MODEL // HG_HEADS
HG_KW = HG_HEADS * HG_DK
HG_VW = HG_HEADS * HG_DV
HG_CHUNK = 64

NSA_HEADS = 16
NSA_KV_HEADS = 4
NSA_GROUP = NSA_HEADS // NSA_KV_HEADS
NSA_DIM = 64
NSA_QW = NSA_HEADS * NSA_DIM
NSA_KVW = NSA_KV_HEADS * NSA_DIM
CMP_LEN = 32
CMP_STRIDE = 16
CMP_HIDDEN = 256
SEL_BLOCK = 64
SEL_TOPN = 8
SEL_FORCE = 1000.0
WINDOW = 512
NSA_Q_BLOCK = 32
ROPE_THETA = 10000.0

D_FF = ((8 * D_MODEL + 3 * 256 - 1) // (3 * 256)) * 256

DN_ALPHA = (2.0 * DEPTH) ** 0.25
DN_BETA = (8.0 * DEPTH) ** -0.25
LN_EPS = 1e-5
RMS_EPS = 1e-6

IN_SIZES = (HG_KW, HG_KW, HG_VW, HG_VW,
            NSA_QW, NSA_KVW, NSA_KVW, NSA_KVW, NSA_KVW, NSA_KVW, NSA_KVW,
            3 * NSA_HEADS, D_MODEL, D_MODEL)
IN_SPLITS = tuple(int(v) for v in np.cumsum(IN_SIZES)[:-1])
N_IN = int(sum(IN_SIZES))

kernel_name = 'hybrid_hgrn2_nsa_deepnorm_block'


def layer_norm(x, g, b):
    x = x.astype(jnp.float32)
    mu = jnp.mean(x, -1, keepdims=True)
    var = jnp.mean(jnp.square(x - mu), -1, keepdims=True)
    return (x - mu) * lax.rsqrt(var + LN_EPS) * g.astype(jnp.float32) + b.astype(jnp.float32)


def rope(x, positions):
    half = x.shape[-1] // 2
    inv_freq = ROPE_THETA ** (-jnp.arange(half, dtype=jnp.float32) / half)
    ang = positions.astype(jnp.float32)[..., None] * inv_freq
    cos = jnp.cos(ang)[:, :, None, :]
    sin = jnp.sin(ang)[:, :, None, :]
    x = x.astype(jnp.float32)
    x1, x2 = x[..., :half], x[..., half:]
    return jnp.concatenate([x1 * cos - x2 * sin, x2 * cos + x1 * sin], -1)


def masked_softmax(s, mask):
    s = jnp.where(mask, s, -1e30)
    m = jnp.max(s, -1, keepdims=True)
    e = jnp.where(mask, jnp.exp(s - m), 0.0)
    return e / jnp.maximum(jnp.sum(e, -1, keepdims=True), 1e-30)


def overlap_matrix(n_cmp, n_blk):
    cs = np.arange(n_cmp)[:, None] * CMP_STRIDE
    bs = np.arange(n_blk)[None, :] * SEL_BLOCK
    ov = np.minimum(cs + CMP_LEN, bs + SEL_BLOCK) - np.maximum(cs, bs)
    return (np.clip(ov, 0, None) / CMP_LEN).astype(np.float32)


def hgrn2_mix(q, f_logit, i, g, lb, norm_g):
    f32 = jnp.float32
    B, S, _ = q.shape
    H, C = HG_HEADS, HG_CHUNK
    nc = S // C

    def heads(t, d):
        return t.astype(f32).reshape(B, nc, C, H, d).transpose(0, 3, 1, 2, 4)

    f = lb + (1.0 - lb) * jax.nn.sigmoid(f_logit.astype(f32))
    q_h = heads(q, HG_DK)
    k_h = heads(1.0 - f, HG_DK)
    v_h = heads(i, HG_DV)
    b = jnp.cumsum(heads(jnp.log(f), HG_DK), axis=3)
    b_last = b[:, :, :, -1:, :]
    q_dec = q_h * jnp.exp(b)
    k_inv = k_h * jnp.exp(-b)
    k_end = k_h * jnp.exp(b_last - b)
    causal = jnp.tril(jnp.ones((C, C), dtype=bool))
    attn = jnp.where(causal, jnp.einsum('bhntk,bhnsk->bhnts', q_dec, k_inv), 0.0)
    o_intra = jnp.einsum('bhnts,bhnsv->bhntv', attn, v_h)

    def step(state, xs):
        qd, ke, v, decay = xs
        o = jnp.einsum('bhtk,bhkv->bhtv', qd, state)
        state = decay[..., None] * state + jnp.einsum('bhsk,bhsv->bhkv', ke, v)
        return state, o

    xs = (jnp.moveaxis(q_dec, 2, 0), jnp.moveaxis(k_end, 2, 0), jnp.moveaxis(v_h, 2, 0),
          jnp.moveaxis(jnp.exp(b_last[:, :, :, 0, :]), 2, 0))
    state0 = jnp.zeros((B, H, HG_DK, HG_DV), f32)
    _, o_inter = lax.scan(step, state0, xs)
    o = o_intra + jnp.moveaxis(o_inter, 0, 2)
    o = o.transpose(0, 2, 3, 1, 4).reshape(B, S, H, HG_DV)
    o = o * lax.rsqrt(jnp.mean(jnp.square(o), -1, keepdims=True) + RMS_EPS) * norm_g.astype(f32)
    return o.reshape(B, S, HG_VW) * jax.nn.silu(g.astype(f32))


def compress(t, pos_emb, w1, b1, w2, b2):
    B, S, H, d = t.shape
    n_cmp = (S - CMP_LEN) // CMP_STRIDE + 1
    idx = np.arange(n_cmp)[:, None] * CMP_STRIDE + np.arange(CMP_LEN)[None, :]
    blk = t[:, idx] + pos_emb[None, None, :, None, :]
    blk = blk.transpose(0, 1, 3, 2, 4).reshape(B, n_cmp, H, CMP_LEN * d)
    hdn = jax.nn.silu(blk @ w1 + b1)
    return hdn @ w2 + b2


def nsa_mix(q, k_cmp, v_cmp, k_sel, v_sel, k_win, v_win, gates):
    f32 = jnp.float32
    B, S = q.shape[:2]
    KV, G, D, QB = NSA_KV_HEADS, NSA_GROUP, NSA_DIM, NSA_Q_BLOCK
    n_blk = S // SEL_BLOCK
    top_n = min(SEL_TOPN, n_blk)
    n_cmp = k_cmp.shape[1]
    cmp_end = jnp.asarray(np.arange(n_cmp) * CMP_STRIDE + CMP_LEN - 1)
    overlap = jnp.asarray(overlap_matrix(n_cmp, n_blk))
    scale = D ** -0.5
    ks_blk = k_sel.reshape(B, n_blk, SEL_BLOCK, KV, D).transpose(0, 3, 1, 2, 4)
    vs_blk = v_sel.reshape(B, n_blk, SEL_BLOCK, KV, D).transpose(0, 3, 1, 2, 4)
    kw_pad = jnp.pad(k_win, ((0, 0), (WINDOW, 0), (0, 0), (0, 0)))
    vw_pad = jnp.pad(v_win, ((0, 0), (WINDOW, 0), (0, 0), (0, 0)))
    gates = jax.nn.sigmoid(gates.astype(f32)).reshape(B, S, 3, KV, G)
    b_idx = jnp.arange(B)[:, None, None, None]
    h_idx = jnp.arange(KV)[None, :, None, None]
    blk_ids = jnp.arange(n_blk)
    sub_pos = jnp.arange(SEL_BLOCK)

    def one_block(i):
        q0 = i * QB
        t = q0 + jnp.arange(QB)
        qb = lax.dynamic_slice_in_dim(q, q0, QB, 1).reshape(B, QB, KV, G, D)
        s_c = jnp.einsum('bqhgd,bnhd->bhgqn', qb, k_cmp) * scale
        p_c = masked_softmax(s_c, cmp_end[None, :] <= t[:, None])
        o_c = jnp.einsum('bhgqn,bnhd->bqhgd', p_c, v_cmp)
        imp = jnp.einsum('bhgqn,nk->bhqk', p_c, overlap)
        cur = t // SEL_BLOCK
        blk_ok = blk_ids[None, :] <= cur[:, None]
        forced = ((blk_ids[None, :] == 0) | (blk_ids[None, :] == cur[:, None])
                  | (blk_ids[None, :] == cur[:, None] - 1))
        score = jnp.where(blk_ok, imp + SEL_FORCE * forced, -1.0)
        _, sel = lax.top_k(score, top_n)
        sel_ok = sel <= cur[None, None, :, None]
        k_g = ks_blk[b_idx, h_idx, sel].reshape(B, KV, QB, top_n * SEL_BLOCK, D)
        v_g = vs_blk[b_idx, h_idx, sel].reshape(B, KV, QB, top_n * SEL_BLOCK, D)
        key_pos = sel[..., None] * SEL_BLOCK + sub_pos
        mask_s = (sel_ok[..., None] & (key_pos <= t[None, None, :, None, None])
                  ).reshape(B, KV, QB, top_n * SEL_BLOCK)
        s_s = jnp.einsum('bqhgd,bhqmd->bhgqm', qb, k_g) * scale
        p_s = masked_softmax(s_s, mask_s[:, :, None])
        o_s = jnp.einsum('bhgqm,bhqmd->bqhgd', p_s, v_g)
        k_wb = lax.dynamic_slice_in_dim(kw_pad, q0, WINDOW + QB, 1)
        v_wb = lax.dynamic_slice_in_dim(vw_pad, q0, WINDOW + QB, 1)
        w_pos = q0 - WINDOW + jnp.arange(WINDOW + QB)
        mask_w = ((w_pos[None, :] >= 0) & (w_pos[None, :] <= t[:, None])
                  & (w_pos[None, :] > t[:, None] - WINDOW))
        s_w = jnp.einsum('bqhgd,bkhd->bhgqk', qb, k_wb) * scale
        p_w = masked_softmax(s_w, mask_w)
        o_w = jnp.einsum('bhgqk,bkhd->bqhgd', p_w, v_wb)
        g = lax.dynamic_slice_in_dim(gates, q0, QB, 1)
        o = (g[:, :, 0, :, :, None] * o_c + g[:, :, 1, :, :, None] * o_s
             + g[:, :, 2, :, :, None] * o_w)
        return o.reshape(B, QB, NSA_QW)

    out = lax.map(one_block, jnp.arange(S // QB))
    return out.transpose(1, 0, 2, 3).reshape(B, S, NSA_QW)


def setup_inputs(seed: int = 0) -> dict:
    key = jax.random.key(seed)
    ks = jax.random.split(key, 32)
    n = lambda k, shape: jax.random.normal(k, shape, jnp.float32)
    L = DEPTH
    cin = CMP_LEN * NSA_DIM
    return {
        'x': n(ks[0], (BATCH, SEQ, D_MODEL)),
        'positions': jnp.broadcast_to(jnp.arange(SEQ, dtype=jnp.int32), (BATCH, SEQ)),
        'w_in': n(ks[1], (L, D_MODEL, N_IN)) * D_MODEL ** -0.5,
        'hg_lb_logits': n(ks[2], (DEPTH + 1, HG_KW)) * 0.5,
        'hg_norm_g': 1.0 + 0.02 * n(ks[3], (L, HG_DV)),
        'cmp_k_pos': 0.02 * n(ks[4], (L, CMP_LEN, NSA_DIM)),
        'cmp_k_w1': n(ks[5], (L, cin, CMP_HIDDEN)) * cin ** -0.5,
        'cmp_k_b1': 0.01 * n(ks[6], (L, CMP_HIDDEN)),
        'cmp_k_w2': n(ks[7], (L, CMP_HIDDEN, NSA_DIM)) * CMP_HIDDEN ** -0.5,
        'cmp_k_b2': 0.01 * n(ks[8], (L, NSA_DIM)),
        'cmp_v_pos': 0.02 * n(ks[9], (L, CMP_LEN, NSA_DIM)),
        'cmp_v_w1': n(ks[10], (L, cin, CMP_HIDDEN)) * cin ** -0.5,
        'cmp_v_b1': 0.01 * n(ks[11], (L, CMP_HIDDEN)),
        'cmp_v_w2': n(ks[12], (L, CMP_HIDDEN, NSA_DIM)) * CMP_HIDDEN ** -0.5,
        'cmp_v_b2': 0.01 * n(ks[13], (L, NSA_DIM)),
        'w_up_hg': n(ks[14], (L, HG_VW, D_MODEL)) * HG_VW ** -0.5,
        'w_up_nsa': n(ks[15], (L, NSA_QW, D_MODEL)) * NSA_QW ** -0.5,
        'w_o': n(ks[16], (L, D_MODEL, D_MODEL)) * (D_MODEL ** -0.5 * DN_BETA),
        'ln1_g': 1.0 + 0.02 * n(ks[17], (L, D_MODEL)),
        'ln1_b': 0.02 * n(ks[18], (L, D_MODEL)),
        'w_ffn_gate': n(ks[19], (L, D_MODEL, D_FF)) * D_MODEL ** -0.5,
        'w_ffn_up': n(ks[20], (L, D_MODEL, D_FF)) * D_MODEL ** -0.5,
        'w_ffn_down': n(ks[21], (L, D_FF, D_MODEL)) * (D_FF ** -0.5 * DN_BETA),
        'ln2_g': 1.0 + 0.02 * n(ks[22], (L, D_MODEL)),
        'ln2_b': 0.02 * n(ks[23], (L, D_MODEL)),
    }


def reference(x, positions, w_in, hg_lb_logits, hg_norm_g,
              cmp_k_pos, cmp_k_w1, cmp_k_b1, cmp_k_w2, cmp_k_b2,
              cmp_v_pos, cmp_v_w1, cmp_v_b1, cmp_v_w2, cmp_v_b2,
              w_up_hg, w_up_nsa, w_o, ln1_g, ln1_b,
              w_ffn_gate, w_ffn_up, w_ffn_down, ln2_g, ln2_b):
    f32 = jnp.float32
    B, S, _ = x.shape
    lb_table = jnp.cumsum(jax.nn.softmax(hg_lb_logits.astype(f32), axis=0), axis=0)

    def kv_heads(t):
        return t.astype(f32).reshape(B, S, NSA_KV_HEADS, NSA_DIM)

    h = x
    for l in range(DEPTH):
        proj = jnp.einsum('bsd,dc->bsc', h, w_in[l])
        (hg_q, hg_f, hg_i, hg_g, nsa_q, k_c, v_c, k_s, v_s, k_w, v_w,
         nsa_gate, gate_a, gate_b) = jnp.split(proj, IN_SPLITS, axis=-1)

        a = hgrn2_mix(hg_q, hg_f, hg_i, hg_g, lb_table[l], hg_norm_g[l])

        q = rope(nsa_q.reshape(B, S, NSA_HEADS, NSA_DIM), positions)
        k_cmp = compress(rope(kv_heads(k_c), positions), cmp_k_pos[l], cmp_k_w1[l],
                         cmp_k_b1[l], cmp_k_w2[l], cmp_k_b2[l])
        v_cmp = compress(kv_heads(v_c), cmp_v_pos[l], cmp_v_w1[l],
                         cmp_v_b1[l], cmp_v_w2[l], cmp_v_b2[l])
        b_out = nsa_mix(q, k_cmp, v_cmp,
                        rope(kv_heads(k_s), positions), kv_heads(v_s),
                        rope(kv_heads(k_w), positions), kv_heads(v_w), nsa_gate)

        a_d = a @ w_up_hg[l]
        b_d = b_out @ w_up_nsa[l]
        merged = (jax.nn.sigmoid(gate_a.astype(f32)) * a_d
                  + jax.nn.sigmoid(gate_b.astype(f32)) * b_d)
        mix = merged @ w_o[l]
        h = layer_norm(DN_ALPHA * h + mix, ln1_g[l], ln1_b[l]).astype(x.dtype)

        ffn = (jax.nn.silu(h @ w_ffn_gate[l]) * (h @ w_ffn_up[l])) @ w_ffn_down[l]
        h = layer_norm(DN_ALPHA * h + ffn, ln2_g[l], ln2_b[l]).astype(x.dtype)
    return h
```

```python
import numpy as np
import concourse.bass as bass
import concourse.mybir as mybir
from concourse.bass_utils import run_bass_kernel_spmd
from contextlib import ExitStack

F32 = mybir.dt.float32
BF16 = mybir.dt.bfloat16
I32 = mybir.dt.int32
AF = mybir.ActivationFunctionType
ALU = mybir.AluOpType

NCORES = 8
SEQ = 2048
NT = 16
DM = 1024
DFF = 2816
ALPHA = 2.0 ** 0.25
TWO_PI = float(2 * np.pi)


class Reg:
    _n = 0

    def __init__(self, name=None):
        Reg._n += 1
        self.name = (name or "r") + f"_{Reg._n}"
        self.writers = {}
        self.readers = {}
        self.dma_sem = None
        self.dma_total = 0


class Sched:
    ENG = ["pe", "act", "dve", "pool", "sp"]

    def __init__(self, nc, es):
        self.nc = nc
        self.es = es
        self.sem = {e: es.enter_context(nc.semaphore("s_" + e)) for e in self.ENG}
        self.cnt = {e: 0 for e in self.ENG}
        self.waited = {e: {} for e in self.ENG}
        self.stream = {e: [] for e in self.ENG}
        self.stores = {}
        self.dmakeys = {}

    def dma_sem(self, R):
        if R.dma_sem is None:
            R.dma_sem = self.es.enter_context(self.nc.semaphore("d_" + R.name))
        return R.dma_sem

    def _need(self, e, key, sv, waits):
        if e == "pe" and key == "pe":
            return
        if self.waited[e].get(key, 0) >= sv[1]:
            return
        if key not in waits or waits[key][1] < sv[1]:
            waits[key] = sv

    def op(self, e, fn, reads=(), writes=(), dma=None, store=False):
        waits = {}
        for r in reads:
            for k, sv in r.writers.items():
                self._need(e, k, sv, waits)
        for w in writes:
            for k, sv in w.writers.items():
                self._need(e, k, sv, waits)
            for k, sv in w.readers.items():
                self._need(e, k, sv, waits)
        for k, sv in waits.items():
            self.waited[e][k] = sv[1]
        if dma is None:
            self.cnt[e] += 1
            key = e
            sv = (self.sem[e], self.cnt[e])
            inc = (self.sem[e], 1)
        else:
            s = self.dma_sem(dma)
            dma.dma_total += 16
            key = "d_" + dma.name
            sv = (s, dma.dma_total)
            inc = (s, 16)
            self.dmakeys[key] = sv
            if store:
                self.stores[key] = sv
        for r in reads:
            if key not in r.readers or r.readers[key][1] < sv[1]:
                r.readers[key] = sv
        for w in writes:
            w.writers = {key: sv}
            w.readers = {}
        self.stream[e].append((list(waits.values()), fn, inc))

    def barrier(self):
        for e in self.ENG:
            waits = []
            for f in self.ENG:
                if f != e and self.cnt[f] > self.waited[e].get(f, 0):
                    waits.append((self.sem[f], self.cnt[f]))
                    self.waited[e][f] = self.cnt[f]
            for k, sv in self.dmakeys.items():
                if self.waited[e].get(k, 0) < sv[1]:
                    waits.append(sv)
                    self.waited[e][k] = sv[1]
            self.stream[e].append((waits, None, None))

    def finish(self):
        waits = []
        for k, sv in self.stores.items():
            if self.waited["sp"].get(k, 0) < sv[1]:
                waits.append(sv)
        self.stream["sp"].append((waits, None, None))

    def emit(self):
        nc = self.nc
        with nc.Block() as block:
            def mk(e):
                def body(eng):
                    for waits, fn, inc in self.stream[e]:
                        for s, v in waits:
                            eng.wait_ge(s, v)
                        if fn is not None:
                            fn(eng).then_inc(inc[0], inc[1])
                return body
            block.tensor(mk("pe"))
            block.scalar(mk("act"))
            block.vector(mk("dve"))
            block.gpsimd(mk("pool"))
            block.sync(mk("sp"))


class Buf:
    def __init__(self, ap, name):
        self.ap = ap
        self.r = Reg(name)

    def __getitem__(self, k):
        return self.ap[k]


def bmid(a, k):
    return bass.AP(a.tensor, a.offset, [list(a.ap[0]), [0, k], list(a.ap[1])])


def blast(a, k):
    return bass.AP(a.tensor, a.offset, [list(a.ap[0]), list(a.ap[1]), [0, k]])


C_ID, C_TRI, C_TREV, C_CI, C_M8, C_INVF, C_NG, C_B1, C_B2K, C_V01, C_FB2, C_E6, C_E5, C_ONE, NCFA = \
    0, 128, 256, 384, 386, 394, 426, 554, 558, 560, 1072, 1584, 1585, 1586, 1592
B_ID, B_OV, B_E, B_CAUS, B_ANTI, B_ONES, B_POST, B_B2V, B_TRI, B_TREV, B_CI, NCB = \
    0, 128, 160, 2208, 2720, 3232, 3360, 3392, 3456, 3584, 3712, 3720

ARENA_BYTES = 207 * 1024
O_CFA = 0
O_CB = O_CFA + NCFA * 4
O_ROML = O_CB + NCB * 2
O_A = ((O_ROML + 4096 + 1023) // 1024) * 1024
O_B = O_A + 32768
O_XT = O_B + 32768
O_KSW = O_XT + 32768
O_VSW = O_KSW + 16384
O_KCMP = O_VSW + NT * 4 * 2 * 65 * 2
O_VCMP = O_KCMP + 1024
O_C2 = O_VCMP + 512
O_S2 = O_C2 + 4096
O_SCR = O_S2 + 4096


def build(NSEQ=4, dbg=False, stop=9, skip=(), npair=4, ntile=NT, hgcut=99):
    nc = bass.Bass("TRN2", target_bir_lowering=False)

    def din(name, shape, dt=F32):
        return nc.dram_tensor(name, shape, dt, kind="ExternalInput").ap()

    x = din("x", [NSEQ * SEQ, DM])
    pos = din("pos", [NSEQ, 128, NT], I32)
    wkv = din("wkv", [128, 8, 1536])
    wq = din("wq", [128, 8, 1024])
    wng = din("wng", [128, 8, 48])
    whg = din("whg", [8, 128, 8, 512])
    wga = din("wga", [128, 8, 1024])
    wgb = din("wgb", [128, 8, 1024])
    wuh = din("wuh", [128, 8, 1024])
    wun = din("wun", [128, 8, 1024])
    wo = din("wo", [128, 8, 1024])
    wfg = din("wfg", [128, 8, DFF])
    wfu = din("wfu", [128, 8, DFF])
    wfd = din("wfd", [128, 22, 1024])
    w1 = din("w1", [128, 32, 256])
    w2k = din("w2k", [128, 2, 64])
    w2v = din("w2v", [128, 2, 64])
    cfa_d = din("cfa", [128, NCFA])
    cb_d = din("cb", [128, NCB])
    lbrows = din("lbrows", [128, 2048])
    lnp_d = din("lnp", [128, 4096])
    y = nc.dram_tensor("y", [NSEQ * SEQ, DM], F32, kind="ExternalOutput").ap()
    dbg_out = {}
    if dbg:
        for nm in ("d_aT", "d_bT"):
            dbg_out[nm] = nc.dram_tensor(nm, [128, 8 * SEQ], F32, kind="ExternalOutput").ap()
        dbg_out["d_h1"] = nc.dram_tensor("d_h1", [SEQ, DM], F32, kind="ExternalOutput").ap()

    es = ExitStack()
    with es:
        S = Sched(nc, es)
        arena = es.enter_context(nc.sbuf_tensor("arena", [128, ARENA_BYTES // 2], BF16))
        arena32 = arena.bitcast(F32)
        arenaI = arena.bitcast(I32)
        psf = [es.enter_context(nc.psum_tensor(f"ps{i}", [128, 512], F32)) for i in range(8)]
        PB = [Buf(p[:, :], f"ps{i}") for i, p in enumerate(psf)]
        PBb = [p.bitcast(BF16)[:, :] for p in psf]

        _bufs = {}

        def at(off, n, dt, name):
            key = (off, n, str(dt), name)
            if key not in _bufs:
                _bufs[key] = _at(off, n, dt, name)
            return _bufs[key]

        def _at(off, n, dt, name):
            assert off % 4 == 0 and off + n * (2 if dt == BF16 else 4) <= ARENA_BYTES, (name, off, n)
            if dt == BF16:
                return Buf(arena[:, off // 2: off // 2 + n], name)
            return Buf((arenaI if dt == I32 else arena32)[:, off // 4: off // 4 + n], name)

        class Cur:
            def __init__(self, off, lim=ARENA_BYTES):
                self.off = off
                self.lim = lim

            def take(self, n, dt, name):
                nb = n * (2 if dt == BF16 else 4)
                b = at(self.off, n, dt, name)
                self.off += (nb + 63) // 64 * 64
                assert self.off <= self.lim, (name, self.off, self.lim)
                return b

        def regs(bs):
            return [b.r if isinstance(b, Buf) else b for b in bs]

        def MM(out, lhsT, rhs, start, stop, R, W, skip=False):
            S.op("pe", lambda e: e.matmul(out, lhsT, rhs, start=start, stop=stop, skip_group_check=skip),
                 regs(R), regs(W))

        def TR(out, in_, ident, R, W):
            S.op("pe", lambda e: e.transpose(out, in_, ident), regs(R), regs(W))

        def ACT(out, in_, func, R, W, bias=None, scale=None, accum=None):
            kw = {}
            if bias is not None:
                kw["bias"] = bias
            if scale is not None:
                kw["scale"] = scale
            if accum is not None:
                kw["accum_out"] = accum
            S.op("act", lambda e: e.activation(out=out, in_=in_, func=func, **kw), regs(R), regs(W))

        def TT(out, in0, in1, op, R, W, eng="dve"):
            S.op(eng, lambda e: e.tensor_tensor(out=out, in0=in0, in1=in1, op=op), regs(R), regs(W))

        def TS(out, in0, s1, s2, op0, op1, R, W, eng="dve"):
            if s2 is None:
                S.op(eng, lambda e: e.tensor_scalar(out=out, in0=in0, scalar1=s1, scalar2=None, op0=op0),
                     regs(R), regs(W))
            else:
                S.op(eng, lambda e: e.tensor_scalar(out=out, in0=in0, scalar1=s1, scalar2=s2, op0=op0, op1=op1),
                     regs(R), regs(W))

        def STT(out, in0, scalar, in1, op0, op1, R, W):
            S.op("dve", lambda e: e.scalar_tensor_tensor(out=out, in0=in0, scalar=scalar, in1=in1, op0=op0, op1=op1),
                 regs(R), regs(W))

        def CPV(out, in_, R, W):
            S.op("dve", lambda e: e.tensor_copy(out, in_), regs(R), regs(W))

        def CPA(out, in_, R, W):
            ACT(out, in_, AF.Copy, R, W)

        def RCP(out, in_, R, W):
            S.op("dve", lambda e: e.reciprocal(out, in_), regs(R), regs(W))

        def MSET(out, val, W, eng="dve"):
            S.op(eng, lambda e: e.memset(out, val), [], regs(W))

        def DMA(q, out, in_, R, W, semb, store=False):
            S.op(q, lambda e: e.dma_start(out=out, in_=in_), regs(R), regs(W), dma=semb.r, store=store)

        cfa = at(O_CFA, NCFA, F32, "cfa")
        cb = at(O_CB, NCB, BF16, "cb")
        roml = at(O_ROML, 1024, F32, "roml")
        bufA = at(O_A, 8 * SEQ, BF16, "A")
        bufB = at(O_B, 8 * SEQ, BF16, "B")
        xT = at(O_XT, 8 * SEQ, BF16, "xT")
        ksw = at(O_KSW, 4 * SEQ, BF16, "ksw")
        vsw = at(O_VSW, NT * 4 * 2 * 65, BF16, "vsw")
        kcmp = at(O_KCMP, 4 * 128, BF16, "kcmp")
        vcmp = at(O_VCMP, 4 * 64, BF16, "vcmp")
        c2 = at(O_C2, NT * 64, F32, "c2")
        s2 = at(O_S2, NT * 64, F32, "s2")
        aTv = bufB.ap.rearrange("p (c n) -> p c n", c=8)
        bTv = bufA.ap.rearrange("p (c n) -> p c n", c=8)
        xTv = xT.ap.rearrange("p (c n) -> p c n", c=8)
        kswv = ksw.ap.rearrange("p (h n) -> p h n", h=4)
        vswv = vsw.ap.rearrange("p (t h b d) -> p t h b d", t=NT, h=4, b=2)
        kcmpv = kcmp.ap.rearrange("p (h n) -> p h n", h=4)
        vcmpv = vcmp.ap.rearrange("p (h d) -> p h d", h=4)
        c2v = c2.ap.rearrange("p (t d) -> p t d", t=NT)
        s2v = s2.ap.rearrange("p (t d) -> p t d", t=NT)
        identf = cfa[:, C_ID:C_ID + 128]
        identb = cb[:, B_ID:B_ID + 128]

        DMA("sp", cfa.ap, cfa_d, [], [cfa], cfa)
        DMA("pool", cb.ap, cb_d, [], [cb], cb)
        ci = Cur(O_SCR)
        lbt = ci.take(2048, F32, "lbt")
        DMA("sp", lbt.ap, lbrows, [], [lbt], lbt)
        TT(lbt[:, 0:1024], lbt[:, 0:1024], lbt[:, 1024:2048], ALU.subtract, [lbt], [lbt])
        ACT(roml.ap, lbt[:, 0:1024], AF.Exp, [lbt], [roml])
        TS(roml.ap, roml.ap, 1.0, None, ALU.add, None, [roml], [roml])

        def rope(src_ps, srcbuf, nh, tile, t1, t2, dst_list):
            sv = src_ps.rearrange("p (h t d) -> p h t d", h=nh, t=2)
            t1v = t1.ap[:, 0:nh * 64].rearrange("p (h d) -> p h d", h=nh)
            t2v = t2.ap[:, 0:nh * 64].rearrange("p (h t d) -> p h t d", h=nh, t=2)
            TT(t1v, src_ps.rearrange("p (h d) -> p h d", h=nh), bmid(c2v[:, tile, :], nh), ALU.mult,
               [srcbuf, c2], [t1])
            TT(t2v[:, :, 0, :], sv[:, :, 1, :], bmid(s2v[:, tile, 0:32], nh), ALU.mult, [srcbuf, s2], [t2])
            TT(t2v[:, :, 1, :], sv[:, :, 0, :], bmid(s2v[:, tile, 32:64], nh), ALU.mult, [srcbuf, s2, t2], [t2])
            t2f = t2.ap[:, 0:nh * 64].rearrange("p (h d) -> p h d", h=nh)
            for dap, dbuf in dst_list:
                TT(dap, t1v, t2f, ALU.add, [t1, t2], [dbuf])

        for s in range(NSEQ):
            tok0 = s * SEQ
            S.barrier()
            ct = at(O_A, 4 * SEQ, BF16, "ct")
            ctv = ct.ap.rearrange("p (h n) -> p h n", h=4)
            w1s = at(O_A + 16384, 32 * 256, BF16, "w1s")
            w1v_ = w1s.ap.rearrange("p (l n) -> p l n", l=32)
            wkvs = at(O_B, 8 * 1536, BF16, "wkvs")
            wkvv = wkvs.ap.rearrange("p (c n) -> p c n", c=8)
            MSET(vsw.ap, 1.0, [vsw])
            DMA("pool", wkvv, wkv, [], [wkvs], wkvs)
            DMA("pool", w1v_, w1, [], [w1s], w1s)
            cu = Cur(O_SCR)
            w2ks = cu.take(128, BF16, "w2ks")
            w2vs = cu.take(128, BF16, "w2vs")
            DMA("pool", w2ks.ap.rearrange("p (h d) -> p h d", h=2), w2k, [], [w2ks], w2ks)
            DMA("pool", w2vs.ap.rearrange("p (h d) -> p h d", h=2), w2v, [], [w2vs], w2vs)
            posi = cu.take(NT, I32, "posi")
            posf = cu.take(NT, F32, "posf")
            ang = cu.take(NT * 32, F32, "ang")
            ang2 = cu.take(NT * 32, F32, "ang2")
            kq = cu.take(NT * 32, F32, "kq")
            kqi = cu.take(NT * 32, I32, "kqi")
            DMA("sp", posi.ap, pos[s], [], [posi], posi)
            CPV(posf.ap, posi.ap, [posi], [posf])
            angv = ang.ap.rearrange("p (t j) -> p t j", t=NT)
            TT(angv, blast(posf.ap, 32), bmid(cfa[:, C_INVF:C_INVF + 32], NT), ALU.mult, [posf, cfa], [ang])
            TS(ang2.ap, ang.ap, float(np.pi / 2), None, ALU.add, None, [ang], [ang2])
            for src, which in ((ang, 0), (ang2, 1)):
                TS(kq.ap, src.ap, 1.0 / TWO_PI, None, ALU.mult, None, [src], [kq])
                CPV(kqi.ap, kq.ap, [kq], [kqi])
                CPV(kq.ap, kqi.ap, [kqi], [kq])
                STT(src.ap, kq.ap, -TWO_PI, src.ap, ALU.mult, ALU.add, [kq, src], [src])
                TS(src.ap, src.ap, 3.1415925, -3.1415925, ALU.min, ALU.max, [src], [src])
                srcv = src.ap.rearrange("p (t j) -> p t j", t=NT)
                if which == 0:
                    ACT(s2v[:, :, 32:64], srcv, AF.Sin, [src], [s2])
                    TS(s2v[:, :, 0:32], s2v[:, :, 32:64], -1.0, None, ALU.mult, None, [s2], [s2])
                else:
                    ACT(c2v[:, :, 0:32], srcv, AF.Sin, [src], [c2])
                    CPV(c2v[:, :, 32:64], c2v[:, :, 0:32], [c2], [c2])
            xin = [cu.take(1024, F32, f"xin{i}") for i in range(2)]
            t1 = cu.take(256, F32, "t1")
            t2 = cu.take(256, F32, "t2")
            krc = cu.take(256, BF16, "krc")
            krcv = krc.ap.rearrange("p (h d) -> p h d", h=4)
            for i in range(ntile if 1 in skip else NT):
                xb = xin[i % 2]
                DMA("sp", xb.ap, x[tok0 + i * 128: tok0 + (i + 1) * 128, :], [], [xb], xb)
                for c in range(8):
                    pb = PB[c // 4]
                    TR(pb[:, (c % 4) * 128:(c % 4 + 1) * 128], xb[:, c * 128:(c + 1) * 128], identf, [xb, cfa], [pb])
                CPA(xTv[:, 0:4, i * 128:(i + 1) * 128], PB[0].ap.rearrange("p (c n) -> p c n", c=4), [PB[0]], [xT])
                CPV(xTv[:, 4:8, i * 128:(i + 1) * 128], PB[1].ap.rearrange("p (c n) -> p c n", c=4), [PB[1]], [xT])
                for hk in range(4):
                    pb = PB[2 + hk % 2]
                    for c in range(8):
                        MM(pb[:, 0:384], xTv[:, c, i * 128:(i + 1) * 128], wkvv[:, c, hk * 384:(hk + 1) * 384],
                           c == 0, c == 7, [xT, wkvs], [pb])
                    rope(pb[:, 0:192], pb, 3, i, t1, t2, [(krcv[:, 0:3, :], krc)])
                    CPA(krc[:, 192:256], pb[:, 192:256], [pb], [krc])
                    CPA(vswv[:, i, hk, :, 0:64], pb[:, 256:384].rearrange("p (b d) -> p b d", b=2), [pb], [vsw])
                    TR(PBb[7][:, 0:128], krc[:, 0:128], identb, [krc, cb], [PB[7]])
                    TR(PBb[7][:, 128:256], krc[:, 128:256], identb, [krc, cb], [PB[7]])
                    CPV(kswv[:, hk, i * 128:(i + 1) * 128], PBb[7][:, 0:128], [PB[7]], [ksw])
                    CPA(ctv[:, hk, i * 128:(i + 1) * 128], PBb[7][:, 128:256], [PB[7]], [ct])
            c1 = cu.take(4, F32, "c1")
            hdn = cu.take(4 * 128, BF16, "hdn")
            hdnv = hdn.ap.rearrange("p (a n) -> p a n", a=4)
            for kv in range(2):
                pb = PB[kv]
                pr = slice(64 * kv, 64 * kv + 64)
                for half in range(2):
                    col = kv * 2 + half
                    for l in range(32):
                        MM(pb[:, col:col + 1], w1v_[pr, l, half * 128:(half + 1) * 128],
                           cb[pr, B_POST + l:B_POST + l + 1], l == 0, l == 31, [w1s, cb], [pb])
                TT(c1[:, 2 * kv:2 * kv + 2], pb[:, 2 * kv:2 * kv + 2], cfa[:, C_B1 + 2 * kv:C_B1 + 2 * kv + 2], ALU.add,
                   [pb, cfa], [c1])
            for hk in range(0 if 1 in skip else 4):
                for kv in range(2):
                    pr = slice(64 * kv, 64 * kv + 64)
                    for half in range(2):
                        col = kv * 2 + half
                        pb = PB[1 + (col % 2)]
                        for l in range(32):
                            MM(pb[:, 0:127], w1v_[pr, l, half * 128:(half + 1) * 128],
                               ctv[pr, hk, l:l + 2017:16], l == 0, l == 31, [w1s, ct], [pb])
                        ACT(hdnv[:, col, 0:127], pb[:, 0:127], AF.Silu, [pb, c1], [hdn], bias=c1[:, col:col + 1])
                pb = PB[3]
                w2kv = w2ks.ap.rearrange("p (h d) -> p h d", h=2)
                w2vv = w2vs.ap.rearrange("p (h d) -> p h d", h=2)
                for half in range(2):
                    MM(pb[0:64, 0:127], w2kv[:, half, :], hdnv[:, half, 0:127], half == 0, half == 1, [w2ks, hdn], [pb])
                ACT(kcmpv[0:64, hk, 0:127], pb[0:64, 0:127], AF.Identity, [pb, cfa], [kcmp],
                    bias=cfa[0:64, C_B2K:C_B2K + 1])
                pb = PB[4]
                for half in range(2):
                    MM(pb[0:127, 0:64], hdnv[:, 2 + half, 0:127], w2vv[:, half, :], half == 0, False, [w2vs, hdn], [pb])
                MM(pb[0:127, 0:64], cb[0:1, B_ONES:B_ONES + 127], cb[0:1, B_B2V:B_B2V + 64], False, True, [cb], [pb])
                CPV(vcmpv[0:127, hk, :], pb[0:127, 0:64], [pb], [vcmp])

            if stop < 2:
                continue
            S.barrier()
            cu = Cur(O_SCR)
            wqs = cu.take(8 * 1024, BF16, "wqs")
            wqv = wqs.ap.rearrange("p (c n) -> p c n", c=8)
            wngs = cu.take(8 * 48, BF16, "wngs")
            wngv = wngs.ap.rearrange("p (c n) -> p c n", c=8)
            DMA("pool", wqv, wq, [], [wqs], wqs)
            DMA("pool", wngv, wng, [], [wngs], wngs)
            t1 = cu.take(256, F32, "t1a")
            t2 = cu.take(256, F32, "t2a")
            qr2 = cu.take(512, BF16, "qr2")
            qr2v = qr2.ap.rearrange("p (g b d) -> p g b d", g=4, b=2)
            qt2 = cu.take(512, BF16, "qt2")
            qt2v = qt2.ap.rearrange("p (g n) -> p g n", g=4)
            ec = cu.take(512, F32, "ec")
            ecv = ec.ap.rearrange("p (g n) -> p g n", g=4)
            pcb = cu.take(512, BF16, "pcb")
            pcbv = pcb.ap.rearrange("p (g n) -> p g n", g=4)
            pct = cu.take(512, BF16, "pct")
            pctv = pct.ap.rearrange("p (g n) -> p g n", g=4)
            st = cu.take(64, F32, "st")
            sc = cu.take(32, F32, "sc")
            nsl = cu.take(32, BF16, "nsl")
            rb = cu.take(512, BF16, "rb")
            rbv = rb.ap.rearrange("p (g n) -> p g n", g=4)
            pt = [cu.take(512, BF16, f"pt{i}") for i in range(2)]
            sg = cu.take(48, F32, "sg")
            oc = cu.take(256, F32, "oc")
            fac = cu.take(8, F32, "fac")
            btile = cu.take(256, BF16, "btile")
            acc = cu.take(256, F32, "acc")
            tmp = cu.take(256, F32, "tmpc")
            Ev = cb[0:32, B_E:B_E + 2048].rearrange("p (j n) -> p j n", j=16)
            qt2s = [qt2, cu.take(512, BF16, "qt2b")]
            rbs = [rb, cu.take(512, BF16, "rbb")]
            ocs = [oc, cu.take(256, F32, "ocb")]
            sgs = [sg, cu.take(48, F32, "sgb")]
            iters = [(i, hk) for i in range(0 if 2 in skip else NT) for hk in range(4)]

            def att_front(n):
                i, hk = iters[n]
                p = n % 2
                tsl = slice(i * 128, (i + 1) * 128)
                ncv = min(127, 8 * i + 7)
                qt2_, rb_, oc_, sg_ = qt2s[p], rbs[p], ocs[p], sgs[i % 2]
                qt2v_ = qt2_.ap.rearrange("p (g n) -> p g n", g=4)
                rbv_ = rb_.ap.rearrange("p (g n) -> p g n", g=4)
                if hk == 0:
                    pb = PB[0]
                    for c in range(8):
                        MM(pb[:, 0:48], xTv[:, c, tsl], wngv[:, c, :], c == 0, c == 7, [xT, wngs], [pb])
                    ACT(sg_.ap, pb[:, 0:48], AF.Exp, [pb], [sg_], scale=-1.0)
                    TS(sg_.ap, sg_.ap, 1.0, None, ALU.add, None, [sg_], [sg_])
                    RCP(sg_.ap, sg_.ap, [sg_], [sg_])
                pb = PB[1]
                for c in range(8):
                    MM(pb[:, 0:256], xTv[:, c, tsl], wqv[:, c, hk * 256:(hk + 1) * 256], c == 0, c == 7,
                       [xT, wqs], [pb])
                rope(pb[:, 0:256], pb, 4, i, t1, t2, [(qr2v[:, :, 0, :], qr2), (qr2v[:, :, 1, :], qr2)])
                yield
                for g in range(4):
                    TR(PBb[7][:, g * 128:(g + 1) * 128], qr2[:, g * 128:(g + 1) * 128], identb, [qr2, cb], [PB[7]])
                CPA(qt2_.ap, PBb[7][:, 0:512], [PB[7]], [qt2_])
                yield
                ps = PB[0]
                psv = ps.ap.rearrange("p (g n) -> p g n", g=4)
                for g in range(4):
                    MM(psv[:, g, 0:ncv], qt2v_[0:64, g, :], kcmpv[0:64, hk, 0:ncv], True, True, [qt2_, kcmp], [ps])
                j0 = 1 if i == 0 else 0
                lo = ncv - (8 - j0)
                TT(psv[:, :, lo:ncv], psv[:, :, lo:ncv], bmid(cfa[:, C_M8 + j0:C_M8 + 8], 4), ALU.add,
                   [ps, cfa], [ps])
                S.op("dve", lambda e, o=st[:, 0:4], a=psv[:, :, 0:ncv]: e.tensor_reduce(
                    out=o, in_=a, axis=mybir.AxisListType.X, op=ALU.max), [ps.r], [st.r])
                TS(st[:, 4:8], st[:, 0:4], -1e4, -0.125, ALU.max, ALU.mult, [st], [st])
                MSET(st[:, 8:12], 0.0, [st])
                for g in range(4):
                    ACT(ecv[:, g, 0:ncv], psv[:, g, 0:ncv], AF.Exp, [ps, st], [ec, st],
                        bias=st[:, 4 + g:5 + g], scale=0.125, accum=st[:, 8 + g:9 + g])
                TS(st[:, 12:16], st[:, 8:12], 1e-30, None, ALU.max, None, [st], [st])
                RCP(st[:, 12:16], st[:, 12:16], [st], [st])
                TT(pcbv[:, :, 0:ncv], ecv[:, :, 0:ncv], blast(st[:, 12:16], ncv), ALU.mult, [ec, st], [pcb])
                yield
                for g in range(4):
                    TR(PBb[7][0:ncv, g * 128:(g + 1) * 128], pcbv[:, g, 0:ncv], identb, [pcb, cb], [PB[7]])
                CPA(pctv[0:ncv, :, :], PBb[7][0:ncv, 0:512].rearrange("p (g n) -> p g n", g=4), [PB[7]], [pct])
                yield
                po = PB[6]
                for g in range(4):
                    MM(po[:, 256:288], pctv[0:ncv, g, :], cb[0:ncv, B_OV:B_OV + 32], g == 0, g == 3,
                       [pct, cb], [po], skip=True)
                for g in range(4):
                    MM(po[:, g * 64:(g + 1) * 64], pctv[0:ncv, g, :], vcmpv[0:ncv, hk, :], False, True,
                       [pct, vcmp], [po], skip=True)
                TT(sc.ap, po[:, 256:288], cfa[:, C_V01 + i * 32:C_V01 + (i + 1) * 32], ALU.mult, [po, cfa], [sc])
                TT(sc.ap, sc.ap, cfa[:, C_FB2 + i * 32:C_FB2 + (i + 1) * 32], ALU.add, [sc, cfa], [sc])
                S.op("dve", lambda e, o=st[:, 16:24], a=sc.ap: e.max(out=o, in_=a), [sc.r], [st.r])
                TS(sc.ap, sc.ap, st[:, 23:24], 30000.0, ALU.is_ge, ALU.mult, [sc, st], [sc])
                TS(nsl.ap, sc.ap, -30000.0, None, ALU.add, None, [sc], [nsl])
                ocv = oc_.ap.rearrange("p (g d) -> p g d", g=4)
                TT(ocv, po[:, 0:256].rearrange("p (g d) -> p g d", g=4), blast(sg_[:, hk * 4:hk * 4 + 4], 64),
                   ALU.mult, [po, sg_], [oc_])
                yield
                TR(PBb[7][0:32, 0:128], nsl.ap, identb, [nsl, cb], [PB[7]])
                CPA(rbv_[0:32, :, :], bmid(PBb[7][0:32, 0:128], 4), [PB[7]], [rb_])

            ptc = [0]

            def att_back(n, gen_next):
                i, hk = iters[n]
                p = n % 2
                tsl = slice(i * 128, (i + 1) * 128)
                qt2_, rb_, oc_, sg_ = qt2s[p], rbs[p], ocs[p], sgs[i % 2]
                pos_ = PB[4]
                pow_ = PB[5]
                jlo = max(0, i - 4)
                items = [("s", j) for j in range(i + 1)] + [("w", j) for j in range(jlo, i + 1)]

                def emit_S(m, base):
                    kind, j = items[m]
                    ps = PB[2 + ((base + m) % 2)]
                    if kind == "s":
                        MM(ps.ap, kswv[0:64, hk, j * 128:(j + 1) * 128], qt2_[0:64, :], True, False, [ksw, qt2_], [ps])
                        MM(ps.ap, Ev[:, j, :], rb_[0:32, :], False, j != i, [cb, rb_], [ps])
                        if j == i:
                            MM(ps.ap, identb, cb[:, B_CAUS:B_CAUS + 512], False, True, [cb], [ps])
                    else:
                        msk = None
                        if j == i:
                            msk = B_CAUS
                        elif j == i - 4:
                            msk = B_ANTI
                        MM(ps.ap, kswv[64:128, hk, j * 128:(j + 1) * 128], qt2_[64:128, :], True, msk is None,
                           [ksw, qt2_], [ps])
                        if msk is not None:
                            MM(ps.ap, identb, cb[:, msk:msk + 512], False, True, [cb], [ps])

                def emit_EXP_PV(m, base):
                    kind, j = items[m]
                    ps = PB[2 + ((base + m) % 2)]
                    ptb = pt[(base + m) % 2]
                    ACT(ptb.ap, ps.ap, AF.Exp, [ps], [ptb], scale=0.125)
                    if kind == "s":
                        for g in range(4):
                            MM(pos_[:, g * 65:(g + 1) * 65], ptb[:, g * 128:(g + 1) * 128], vswv[:, j, hk, 0, :],
                               (j == 0 and g == 0), j == i, [ptb, vsw], [pos_], skip=True)
                    else:
                        for g in range(4):
                            MM(pow_[:, g * 65:(g + 1) * 65], ptb[:, g * 128:(g + 1) * 128], vswv[:, j, hk, 1, :],
                               (j == jlo and g == 0), j == i, [ptb, vsw], [pow_], skip=True)

                base = ptc[0]
                stride = max(1, len(items) // 6)
                emit_S(0, base)
                for m in range(len(items)):
                    if m + 1 < len(items):
                        emit_S(m + 1, base)
                    emit_EXP_PV(m, base)
                    if gen_next is not None and m % stride == stride - 1:
                        next(gen_next, None)
                ptc[0] += len(items)
                if gen_next is not None:
                    for _ in gen_next:
                        pass
                accv = acc.ap.rearrange("p (g d) -> p g d", g=4)
                tmpv = tmp.ap.rearrange("p (g d) -> p g d", g=4)
                for br, pacc in ((1, pos_), (2, pow_)):
                    pv = pacc[:, 0:260].rearrange("p (g d) -> p g d", g=4)
                    TS(fac[:, 0:4], pv[:, :, 64], 1e-30, None, ALU.max, None, [pacc], [fac])
                    RCP(fac[:, 0:4], fac[:, 0:4], [fac], [fac])
                    TT(fac[:, 4:8], fac[:, 0:4], sg_[:, br * 16 + hk * 4:br * 16 + hk * 4 + 4], ALU.mult,
                       [fac, sg_], [fac])
                    TT(tmpv, pv[:, :, 0:64], blast(fac[:, 4:8], 64), ALU.mult, [pacc, fac], [tmp])
                    if br == 1:
                        TT(acc.ap, tmp.ap, oc_.ap, ALU.add, [tmp, oc_], [acc])
                    else:
                        TT(btile.ap, tmp.ap, acc.ap, ALU.add, [tmp, acc], [btile])
                for hh in range(2):
                    TR(PBb[7][:, hh * 128:(hh + 1) * 128], btile[:, hh * 128:(hh + 1) * 128], identb,
                       [btile, cb], [PB[7]])
                CPV(bTv[:, hk * 2:hk * 2 + 2, tsl], PBb[7][:, 0:256].rearrange("p (c n) -> p c n", c=2),
                    [PB[7]], [bufA])

            if iters:
                for _ in att_front(0):
                    pass
                for n in range(len(iters)):
                    att_back(n, att_front(n + 1) if n + 1 < len(iters) else None)

            if stop < 3:
                continue
            S.barrier()
            cu = Cur(O_SCR)
            wh = [cu.take(8 * 512, BF16, f"wh{k}") for k in range(4)]

            def load_wh(h):
                b = wh[h % 4]
                DMA("pool", b.ap.rearrange("p (c n) -> p c n", c=8), whg[h], [], [b], b)

            def f32t(nm):
                return [cu.take(128, F32, f"{nm}{k}") for k in range(2)]

            def b16t(nm):
                return [cu.take(128, BF16, f"{nm}{k}") for k in range(2)]

            e1, kk, logf, eb, gs, sq = f32t("e1"), f32t("kk"), f32t("logf"), f32t("eb"), f32t("gs"), f32t("sq")
            qd, ki, ke, vv, attn, qd0, qd1, kit, ab = (b16t("qd"), b16t("ki"), b16t("ke"), b16t("vv"),
                                                       b16t("attn"), b16t("qd0"), b16t("qd1"), b16t("kit"), b16t("ab"))
            dec = f32t("dec")
            lhi, llo = b16t("lhi"), b16t("llo")
            sst = f32t("sst")
            Sf = f32t("Sf")
            Sb0, Sb1 = b16t("Sb0"), b16t("Sb1")
            for k in range(2):
                MSET(qd0[k].ap, 0.0, [qd0[k]])
                MSET(qd1[k].ap, 0.0, [qd1[k]])
            load_wh(0)
            load_wh(1)
            tri = cfa[:, C_TRI:C_TRI + 128]
            trev = cfa[:, C_TREV:C_TREV + 128]
            for pair in range(npair):
                if pair < 3:
                    load_wh(2 * pair + 2)
                    load_wh(2 * pair + 3)
                for k in range(2):
                    MSET(Sf[k].ap, 0.0, [Sf[k]])
                    MSET(Sb0[k].ap, 0.0, [Sb0[k]])
                def hg_proj(i_, k_):
                    h_ = 2 * pair + k_
                    whv_ = wh[h_ % 4].ap.rearrange("p (c n) -> p c n", c=8)
                    for c in range(8):
                        MM(PB[k_].ap, xTv[:, c, i_ * 128:(i_ + 1) * 128], whv_[:, c, :], c == 0, c == 7,
                           [xT, wh[h_ % 4]], [PB[k_]])

                hg_proj(0, 0)
                for i in range(ntile):
                    tsl = slice(i * 128, (i + 1) * 128)
                    for k in range(2):
                        h = 2 * pair + k
                        pp = PB[k]
                        if k == 0:
                            hg_proj(i, 1)
                        elif i + 1 < ntile:
                            hg_proj(i + 1, 0)
                        if hgcut < 1:
                            continue
                        rm = roml[:, h * 128:(h + 1) * 128]
                        ACT(e1[k].ap, pp[:, 128:256], AF.Exp, [pp], [e1[k]])
                        STT(e1[k].ap, e1[k].ap, 1.0, rm, ALU.add, ALU.mult, [e1[k], roml], [e1[k]])
                        RCP(kk[k].ap, e1[k].ap, [e1[k]], [kk[k]])
                        if hgcut < 2:
                            continue
                        ACT(logf[k].ap, kk[k].ap, AF.Ln, [kk[k], cfa], [logf[k]], scale=-1.0, bias=cfa[:, C_ONE:C_ONE + 1])
                        if hgcut < 3:
                            continue
                        pc_ = PB[2 + k]
                        CPA(lhi[k].ap, logf[k].ap, [logf[k]], [lhi[k]])
                        TT(llo[k].ap, logf[k].ap, lhi[k].ap, ALU.subtract, [logf[k], lhi[k]], [llo[k]])
                        if hgcut < 4:
                            continue
                        trib = cb[:, B_TRI:B_TRI + 128]
                        trevb = cb[:, B_TREV:B_TREV + 128]
                        cib = cb[:, B_CI:B_CI + 2]
                        MM(pc_[:, 0:128], trib, lhi[k].ap, True, False, [cb, lhi[k]], [pc_])
                        MM(pc_[:, 0:128], trib, llo[k].ap, False, True, [cb, llo[k]], [pc_])
                        MM(pc_[:, 128:256], trevb, lhi[k].ap, True, False, [cb, lhi[k]], [pc_])
                        MM(pc_[:, 128:256], trevb, llo[k].ap, False, True, [cb, llo[k]], [pc_])
                        if hgcut < 5:
                            continue
                        MM(pc_[:, 256:258], lhi[k].ap, cib, True, False, [cb, lhi[k]], [pc_])
                        MM(pc_[:, 256:258], llo[k].ap, cib, False, True, [cb, llo[k]], [pc_])
                        if hgcut < 6:
                            continue
                        ACT(eb[k].ap, pc_[:, 0:128], AF.Exp, [pc_], [eb[k]])
                        TT(qd[k].ap, pp[:, 0:128], eb[k].ap, ALU.mult, [pp, eb[k]], [qd[k]])
                        ACT(eb[k].ap, pc_[:, 0:128], AF.Exp, [pc_, qd[k]], [eb[k]], scale=-1.0)
                        TT(ki[k].ap, kk[k].ap, eb[k].ap, ALU.mult, [kk[k], eb[k]], [ki[k]])
                        ACT(eb[k].ap, pc_[:, 128:256], AF.Exp, [pc_, ki[k]], [eb[k]])
                        TT(ke[k].ap, kk[k].ap, eb[k].ap, ALU.mult, [kk[k], eb[k]], [ke[k]])
                        if hgcut < 7:
                            continue
                        ACT(dec[k][:, 0:2], pc_[:, 256:258], AF.Exp, [pc_], [dec[k]])
                        CPA(vv[k].ap, pp[:, 256:384], [pp], [vv[k]])
                        if hgcut < 8:
                            continue
                        ACT(gs[k].ap, pp[:, 384:512], AF.Exp, [pp], [gs[k]], scale=-1.0)
                        TS(gs[k].ap, gs[k].ap, 1.0, None, ALU.add, None, [gs[k]], [gs[k]])
                        RCP(gs[k].ap, gs[k].ap, [gs[k]], [gs[k]])
                        TT(gs[k].ap, gs[k].ap, cfa[:, C_NG:C_NG + 128], ALU.mult, [gs[k], cfa], [gs[k]])
                        TT(gs[k].ap, pp[:, 384:512], gs[k].ap, ALU.mult, [gs[k], pp], [gs[k]])
                        if hgcut < 9:
                            continue
                        pt_ = PB[7]
                        TR(PBb[7][:, 0:128], qd[k].ap, identb, [qd[k], cb], [pt_])
                        TR(PBb[7][:, 128:256], ki[k].ap, identb, [ki[k], cb], [pt_])
                        CPA(qd0[k][:, 0:64], PBb[7][:, 0:64], [pt_], [qd0[k]])
                        CPV(qd1[k][:, 64:128], PBb[7][:, 64:128], [pt_], [qd1[k]])
                        CPA(kit[k].ap, PBb[7][:, 128:256], [pt_], [kit[k]])
                        if hgcut < 10:
                            continue
                        pa = PB[4 + k]
                        MM(pa[:, 0:128], kit[k].ap, qd0[k].ap, True, False, [kit[k], qd0[k]], [pa])
                        MM(pa[:, 0:128], kit[k].ap, qd1[k].ap, False, True, [kit[k], qd1[k]], [pa])
                        TT(attn[k].ap, pa[:, 0:128], tri, ALU.mult, [pa, cfa], [attn[k]])
                        if hgcut < 11:
                            continue
                        pu = PB[6]
                        MM(pu[:, 0:128], ke[k][0:64, :], vv[k][0:64, :], True, True, [ke[k], vv[k]], [pu])
                        MM(pc_[:, 384:512], ke[k][64:128, :], vv[k][64:128, :], True, True, [ke[k], vv[k]], [pc_])
                        if hgcut < 11.2:
                            continue
                        TS(Sf[k].ap, Sf[k].ap, dec[k][:, 0:1], None, ALU.mult, None, [Sf[k], dec[k]], [Sf[k]])
                        TT(Sf[k].ap, pu[:, 0:128], Sf[k].ap, ALU.add, [Sf[k], pu], [Sf[k]])
                        CPA(Sb1[k].ap, Sf[k].ap, [Sf[k]], [Sb1[k]])
                        if hgcut < 11.4:
                            continue
                        MM(pa[:, 128:256], attn[k].ap, vv[k].ap, True, False, [attn[k], vv[k]], [pa])
                        MM(pa[:, 128:256], qd0[k].ap, Sb0[k].ap, False, False, [qd0[k], Sb0[k]], [pa])
                        MM(pa[:, 128:256], qd1[k].ap, Sb1[k].ap, False, True, [qd1[k], Sb1[k]], [pa])
                        if hgcut < 11.6:
                            continue
                        TS(Sf[k].ap, Sf[k].ap, dec[k][:, 1:2], None, ALU.mult, None, [Sf[k], dec[k]], [Sf[k]])
                        TT(Sf[k].ap, pc_[:, 384:512], Sf[k].ap, ALU.add, [Sf[k], pc_], [Sf[k]])
                        CPA(Sb0[k].ap, Sf[k].ap, [Sf[k]], [Sb0[k]])
                        if hgcut < 12:
                            continue
                        MSET(sst[k][:, 0:1], 0.0, [sst[k]])
                        ACT(sq[k].ap, pa[:, 128:256], AF.Square, [pa, sst[k]], [sq[k], sst[k]], accum=sst[k][:, 0:1])
                        if hgcut < 13:
                            continue
                        ACT(sst[k][:, 1:2], sst[k][:, 0:1], AF.Ln, [sst[k], cfa], [sst[k]], scale=1.0 / 128, bias=cfa[:, C_E6:C_E6 + 1])
                        ACT(sst[k][:, 2:3], sst[k][:, 1:2], AF.Exp, [sst[k]], [sst[k]], scale=-0.5)
                        if hgcut < 14:
                            continue
                        STT(ab[k].ap, pa[:, 128:256], sst[k][:, 2:3], gs[k].ap, ALU.mult, ALU.mult,
                            [pa, sst[k], gs[k]], [ab[k]])
                        TR(PBb[7][:, 256:384], ab[k].ap, identb, [ab[k], cb], [pt_])
                        CPV(aTv[:, h, tsl], PBb[7][:, 256:384], [pt_], [bufB])

            if dbg and s == 0:
                S.barrier()
                for nm, b in (("d_aT", bufB), ("d_bT", bufA)):
                    dtmp = at(O_XT, 8192, F32, "dtmp" + nm)
                    for q in range(2):
                        CPV(dtmp.ap, b[:, q * 8192:(q + 1) * 8192], [b], [dtmp])
                        DMA("sp", dbg_out[nm][:, q * 8192:(q + 1) * 8192], dtmp.ap, [dtmp], [], dtmp, store=True)

            if stop < 4:
                continue
            S.barrier()
            cu = Cur(O_XT)
            xtg = cu.take(8 * 512, BF16, "xtg")
            xtgv = xtg.ap.rearrange("p (c n) -> p c n", c=8)
            hx = [cu.take(1024, F32, f"hx{k}") for k in range(4)]
            h1t = cu.take(8 * 512, BF16, "h1t")
            h1tv = h1t.ap.rearrange("p (c n) -> p c n", c=8)
            mgt = cu.take(8 * 512, BF16, "mgt")
            mgtv = mgt.ap.rearrange("p (c n) -> p c n", c=8)
            actt = cu.take(22 * 512, BF16, "actt")
            acttv = actt.ap.rearrange("p (c n) -> p c n", c=22)
            ring = [cu.take(4096, BF16, f"ring{k}") for k in range(4)]
            sga = cu.take(512, F32, "sga")
            sgb = cu.take(512, F32, "sgb")
            m1 = cu.take(512, F32, "m1")
            m2 = cu.take(512, F32, "m2")
            hb = cu.take(1024, BF16, "hb")
            lnp = cu.take(4096, F32, "lnp")
            lst = cu.take(16, F32, "lst")
            junk = m1
            DMA("sp", lnp.ap, lnp_d, [], [lnp], lnp)
            rc = [0]

            def wload(src3d, ncol_total, c0, ncols, kdim=8):
                b = ring[rc[0] % 4]
                rc[0] += 1
                v = b.ap[:, 0:kdim * ncols].rearrange("p (c n) -> p c n", c=kdim)
                DMA("pool", v, src3d[:, :, c0:c0 + ncols], [], [b], b)
                return b, v

            def layer_norm(src, gcol, bcol, dst_f32, dst_buf):
                MSET(lst[:, 8:12], 0.0, [lst])
                ACT(junk[:, 0:512], src[:, 0:512], AF.Identity, [src, lst], [junk, lst], accum=lst[:, 8:9])
                ACT(junk[:, 0:512], src[:, 512:1024], AF.Identity, [src, lst], [junk, lst], accum=lst[:, 10:11])
                ACT(junk[:, 0:512], src[:, 0:512], AF.Square, [src, lst], [junk, lst], accum=lst[:, 9:10])
                ACT(junk[:, 0:512], src[:, 512:1024], AF.Square, [src, lst], [junk, lst], accum=lst[:, 11:12])
                TT(lst[:, 0:2], lst[:, 8:10], lst[:, 10:12], ALU.add, [lst], [lst])
                TS(lst[:, 2:4], lst[:, 0:2], 1.0 / 1024, None, ALU.mult, None, [lst], [lst])
                TT(lst[:, 4:5], lst[:, 2:3], lst[:, 2:3], ALU.mult, [lst], [lst])
                TT(lst[:, 5:6], lst[:, 3:4], lst[:, 4:5], ALU.subtract, [lst], [lst])
                ACT(lst[:, 6:7], lst[:, 5:6], AF.Ln, [lst, cfa], [lst], bias=cfa[:, C_E5:C_E5 + 1])
                ACT(lst[:, 7:8], lst[:, 6:7], AF.Exp, [lst], [lst], scale=-0.5)
                TS(src.ap, src.ap, lst[:, 2:3], lst[:, 7:8], ALU.subtract, ALU.mult, [src, lst], [src])
                TT(src.ap, src.ap, lnp[:, gcol:gcol + 1024], ALU.mult, [src, lnp], [src])
                TT(dst_f32, src.ap, lnp[:, bcol:bcol + 1024], ALU.add, [src, lnp], [dst_buf])

            for grp in range(4):
                g0 = grp * 512
                gsl = slice(g0, g0 + 512)
                for tt in range(4):
                    i = grp * 4 + tt
                    xb = hx[tt]
                    DMA("sp", xb.ap, x[tok0 + i * 128: tok0 + (i + 1) * 128, :], [], [xb], xb)
                    for c in range(8):
                        pb = PB[c // 4]
                        TR(pb[:, (c % 4) * 128:(c % 4 + 1) * 128], xb[:, c * 128:(c + 1) * 128], identf, [xb, cfa], [pb])
                    CPA(xtgv[:, 0:4, tt * 128:(tt + 1) * 128], PB[0].ap.rearrange("p (c n) -> p c n", c=4), [PB[0]], [xtg])
                    CPV(xtgv[:, 4:8, tt * 128:(tt + 1) * 128], PB[1].ap.rearrange("p (c n) -> p c n", c=4), [PB[1]], [xtg])
                for cb2 in range(2):
                    bga, vga = wload(wga, 1024, cb2 * 512, 512)
                    buh, vuh = wload(wuh, 1024, cb2 * 512, 512)
                    bgb, vgb = wload(wgb, 1024, cb2 * 512, 512)
                    bun, vun = wload(wun, 1024, cb2 * 512, 512)
                    for cc in range(4):
                        csl = slice(cc * 128, (cc + 1) * 128)
                        oc_ = cb2 * 4 + cc
                        qa, qb, qc, qd_ = (PB[2], PB[3], PB[4], PB[5]) if cc % 2 == 0 else (PB[0], PB[1], PB[6], PB[7])
                        for c in range(8):
                            MM(qa.ap, vga[:, c, csl], xtgv[:, c, :], c == 0, c == 7, [bga, xtg], [qa])
                        for c in range(8):
                            MM(qb.ap, vuh[:, c, csl], aTv[:, c, gsl], c == 0, c == 7, [buh, bufB], [qb])
                        for c in range(8):
                            MM(qc.ap, vgb[:, c, csl], xtgv[:, c, :], c == 0, c == 7, [bgb, xtg], [qc])
                        for c in range(8):
                            MM(qd_.ap, vun[:, c, csl], bTv[:, c, gsl], c == 0, c == 7, [bun, bufA], [qd_])
                        ACT(sga.ap, qa.ap, AF.Sigmoid, [qa], [sga])
                        ACT(sgb.ap, qc.ap, AF.Sigmoid, [qc], [sgb])
                        TT(m1.ap, qb.ap, sga.ap, ALU.mult, [qb, sga], [m1])
                        TT(m2.ap, qd_.ap, sgb.ap, ALU.mult, [qd_, sgb], [m2])
                        TT(mgtv[:, oc_, :], m1.ap, m2.ap, ALU.add, [m1, m2], [mgt])
                bo0, vo0 = wload(wo, 1024, 0, 512)
                bo1, vo1 = wload(wo, 1024, 512, 512)
                for tt in range(4):
                    tl = slice(tt * 128, (tt + 1) * 128)
                    for hf, (bo, vo) in enumerate(((bo0, vo0), (bo1, vo1))):
                        pb = PB[6 + hf]
                        for c in range(8):
                            MM(pb.ap, mgtv[:, c, tl], vo[:, c, :], c == 0, c == 7, [mgt, bo], [pb])
                    xb = hx[tt]
                    for hf in range(2):
                        STT(xb[:, hf * 512:(hf + 1) * 512], xb[:, hf * 512:(hf + 1) * 512], ALPHA, PB[6 + hf].ap,
                            ALU.mult, ALU.add, [xb, PB[6 + hf]], [xb])
                    layer_norm(xb, 0, 1024, xb.ap, xb)
                    if dbg and s == 0:
                        i = grp * 4 + tt
                        DMA("sp", dbg_out["d_h1"][i * 128:(i + 1) * 128, :], xb.ap, [xb], [], xb, store=True)
                    CPA(hb.ap, xb.ap, [xb], [hb])
                    for c in range(8):
                        TR(PBb[c // 4][:, (c % 4) * 128:(c % 4 + 1) * 128], hb[:, c * 128:(c + 1) * 128], identb,
                           [hb, cb], [PB[c // 4]])
                    CPA(h1tv[:, 0:4, tl], PBb[0][:, 0:512].rearrange("p (c n) -> p c n", c=4), [PB[0]], [h1t])
                    CPV(h1tv[:, 4:8, tl], PBb[1][:, 0:512].rearrange("p (c n) -> p c n", c=4), [PB[1]], [h1t])
                for fb in range(6):
                    ncol = 512 if fb < 5 else 256
                    bg, vg = wload(wfg, DFF, fb * 512, ncol)
                    bu, vu = wload(wfu, DFF, fb * 512, ncol)
                    for cc in range(ncol // 128):
                        csl = slice(cc * 128, (cc + 1) * 128)
                        fc = fb * 4 + cc
                        pg, pu_ = PB[2 + 2 * (fc % 2)], PB[3 + 2 * (fc % 2)]
                        for c in range(8):
                            MM(pg.ap, vg[:, c, csl], h1tv[:, c, :], c == 0, c == 7, [bg, h1t], [pg])
                        for c in range(8):
                            MM(pu_.ap, vu[:, c, csl], h1tv[:, c, :], c == 0, c == 7, [bu, h1t], [pu_])
                        ACT(sga.ap, pg.ap, AF.Silu, [pg], [sga])
                        TT(acttv[:, fc, :], sga.ap, pu_.ap, ALU.mult, [sga, pu_], [actt])
                for db in range(6):
                    nk = 4 if db < 5 else 2
                    b = ring[rc[0] % 4]
                    rc[0] += 1
                    v = b.ap[:, 0:nk * 1024].rearrange("p (k n) -> p k n", k=nk)
                    DMA("pool", v, wfd[:, db * 4:db * 4 + nk, :], [], [b], b)
                    for tt in range(4):
                        tl = slice(tt * 128, (tt + 1) * 128)
                        for kq_ in range(nk):
                            kidx = db * 4 + kq_
                            for hf in range(2):
                                pb = PB[tt * 2 + hf]
                                MM(pb.ap, acttv[:, kidx, tl], v[:, kq_, hf * 512:(hf + 1) * 512], kidx == 0, kidx == 21,
                                   [actt, b], [pb])
                for tt in range(4):
                    i = grp * 4 + tt
                    xb = hx[tt]
                    for hf in range(2):
                        pb = PB[tt * 2 + hf]
                        STT(xb[:, hf * 512:(hf + 1) * 512], xb[:, hf * 512:(hf + 1) * 512], ALPHA, pb.ap,
                            ALU.mult, ALU.add, [xb, pb], [xb])
                    layer_norm(xb, 2048, 3072, xb.ap, xb)
                    DMA("sp", y[tok0 + i * 128: tok0 + (i + 1) * 128, :], xb.ap, [xb], [], xb, store=True)

        S.finish()
        S.emit()
    return nc


def _pc(M):
    n = M.shape[1]
    return np.ascontiguousarray(M.reshape(M.shape[0] // 128, 128, n).transpose(1, 0, 2))


def prep_common(inp):
    f32 = np.float32
    W = np.asarray(inp["w_in"], f32)[0]
    o = np.cumsum([0, 1024, 1024, 1024, 1024, 1024, 256, 256, 256, 256, 256, 256, 48, 1024, 1024])
    hq, hf, hi, hg, nq, kc, vc, ks, vs, kw, vw, ng, ga, gb = [int(v) for v in o[:14]]
    d = {}
    d["whg"] = np.stack([_pc(np.concatenate([W[:, hq + h * 128: hq + (h + 1) * 128], W[:, hf + h * 128: hf + (h + 1) * 128],
                                             W[:, hi + h * 128: hi + (h + 1) * 128], W[:, hg + h * 128: hg + (h + 1) * 128]], 1))
                         for h in range(8)])
    cols = []
    for k in range(4):
        sl = lambda b: W[:, b + k * 64: b + (k + 1) * 64]
        cols += [sl(ks), sl(kw), sl(kc), sl(vc), sl(vs), sl(vw)]
    d["wkv"] = _pc(np.concatenate(cols, 1))
    d["wq"] = _pc(W[:, nq:nq + 1024])
    d["wng"] = _pc(W[:, ng:ng + 48])
    d["wga"] = _pc(W[:, ga:ga + 1024])
    d["wgb"] = _pc(W[:, gb:gb + 1024])
    d["wuh"] = _pc(np.asarray(inp["w_up_hg"], f32)[0])
    d["wun"] = _pc(np.asarray(inp["w_up_nsa"], f32)[0])
    d["wo"] = _pc(np.asarray(inp["w_o"], f32)[0])
    d["wfg"] = _pc(np.asarray(inp["w_ffn_gate"], f32)[0])
    d["wfu"] = _pc(np.asarray(inp["w_ffn_up"], f32)[0])
    d["wfd"] = _pc(np.asarray(inp["w_ffn_down"], f32)[0])
    w1k = np.asarray(inp["cmp_k_w1"], f32)[0].reshape(32, 64, 256).transpose(1, 0, 2)
    w1v = np.asarray(inp["cmp_v_w1"], f32)[0].reshape(32, 64, 256).transpose(1, 0, 2)
    d["w1"] = np.ascontiguousarray(np.concatenate([w1k, w1v], 0))
    d["w2k"] = np.ascontiguousarray(np.asarray(inp["cmp_k_w2"], f32)[0].reshape(2, 128, 64).transpose(1, 0, 2))
    d["w2v"] = np.ascontiguousarray(np.asarray(inp["cmp_v_w2"], f32)[0].reshape(2, 128, 64).transpose(1, 0, 2))
    cfa = np.zeros((128, NCFA), f32)
    cfa[:, C_ID:C_ID + 128] = np.eye(128, dtype=f32)
    sidx = np.arange(128)[:, None]
    tidx = np.arange(128)[None, :]
    same = (sidx // 64) == (tidx // 64)
    cfa[:, C_TRI:C_TRI + 128] = (same & (sidx <= tidx)).astype(f32)
    cfa[:, C_TREV:C_TREV + 128] = (same & (sidx > tidx)).astype(f32)
    cfa[:, C_CI] = (np.arange(128) < 64)
    cfa[:, C_CI + 1] = (np.arange(128) >= 64)
    p = np.arange(128)[:, None]
    j = np.arange(8)[None, :]
    cfa[:, C_M8:C_M8 + 8] = np.where(p >= 16 * j + 15, 0.0, -1e30)
    cfa[:, C_INVF:C_INVF + 32] = (10000.0 ** (-np.arange(32, dtype=f32) / 32)).astype(f32)[None, :]
    cfa[:, C_NG:C_NG + 128] = np.asarray(inp["hg_norm_g"], f32)[0][None, :]
    b1k = np.asarray(inp["cmp_k_b1"], f32)[0]
    b1v = np.asarray(inp["cmp_v_b1"], f32)[0]
    cfa[:, C_B1 + 0] = b1k[0:128]
    cfa[:, C_B1 + 1] = b1k[128:256]
    cfa[:, C_B1 + 2] = b1v[0:128]
    cfa[:, C_B1 + 3] = b1v[128:256]
    cfa[0:64, C_B2K] = np.asarray(inp["cmp_k_b2"], f32)[0]
    v01 = np.zeros((128, 16, 32), f32)
    fb2 = np.zeros((128, 16, 32), f32)
    blk = np.arange(32)[None, :]
    for i in range(16):
        cur = (2 * i + (np.arange(128) >= 64))[:, None]
        valid = blk <= cur
        forced = (blk == 0) | (blk == cur) | (blk == cur - 1)
        v01[:, i, :] = valid
        fb2[:, i, :] = np.where(valid, 1000.0 * forced, -1.0)
    cfa[:, C_V01:C_V01 + 512] = v01.reshape(128, 512)
    cfa[:, C_FB2:C_FB2 + 512] = fb2.reshape(128, 512)
    cfa[:, C_E6] = 1e-6
    cfa[:, C_E5] = 1e-5
    cfa[:, C_ONE] = 1.0
    d["cfa"] = cfa
    cb = np.zeros((128, NCB), f32)
    cb[:, B_ID:B_ID + 128] = np.eye(128, dtype=f32)
    cs = np.arange(127)[:, None] * 16
    bs = np.arange(32)[None, :] * 64
    ov = np.clip(np.minimum(cs + 32, bs + 64) - np.maximum(cs, bs), 0, None) / 32.0
    cb[0:127, B_OV:B_OV + 32] = ov
    E = np.zeros((32, 16, 128), f32)
    for jj in range(16):
        for key in range(128):
            E[2 * jj + key // 64, jj, key] = 1.0
    cb[0:32, B_E:B_E + 2048] = E.reshape(32, 2048)
    kq = np.arange(128)
    caus = np.where(kq[:, None] > kq[None, :], -30000.0, 0.0).astype(f32)
    anti = np.where(kq[:, None] <= kq[None, :], -30000.0, 0.0).astype(f32)
    cb[:, B_CAUS:B_CAUS + 512] = np.tile(caus, (1, 4))
    cb[:, B_ANTI:B_ANTI + 512] = np.tile(anti, (1, 4))
    cb[:, B_ONES:B_ONES + 128] = 1.0
    cb[0:64, B_POST:B_POST + 32] = np.asarray(inp["cmp_k_pos"], f32)[0].T
    cb[64:128, B_POST:B_POST + 32] = np.asarray(inp["cmp_v_pos"], f32)[0].T
    cb[:, B_B2V:B_B2V + 64] = np.asarray(inp["cmp_v_b2"], f32)[0][None, :]
    cb[:, B_TRI:B_TRI + 128] = cfa[:, C_TRI:C_TRI + 128]
    cb[:, B_TREV:B_TREV + 128] = cfa[:, C_TREV:C_TREV + 128]
    cb[:, B_CI:B_CI + 2] = cfa[:, C_CI:C_CI + 2]
    d["cb"] = cb
    lb = np.asarray(inp["hg_lb_logits"], f32)
    d["lbrows"] = np.ascontiguousarray(np.broadcast_to(lb.reshape(1, 2048), (128, 2048)))
    lnp = np.concatenate([np.asarray(inp[k], f32)[0] for k in ("ln1_g", "ln1_b", "ln2_g", "ln2_b")])
    d["lnp"] = np.ascontiguousarray(np.broadcast_to(lnp[None, :], (128, 4096)))
    return d


_NC_CACHE = {}


def kernel(**inp):
    x = np.asarray(inp["x"], np.float32)
    pos = np.asarray(inp["positions"], np.int32)
    common = prep_common(inp)
    nseq = x.shape[0] // NCORES
    if nseq not in _NC_CACHE:
        _NC_CACHE[nseq] = build(nseq)
    nc = _NC_CACHE[nseq]
    in_maps = []
    for c in range(NCORES):
        m = dict(common)
        m["x"] = np.ascontiguousarray(x[c * nseq:(c + 1) * nseq].reshape(nseq * SEQ, DM))
        m["pos"] = np.ascontiguousarray(pos[c * nseq:(c + 1) * nseq].reshape(nseq, NT, 128).transpose(0, 2, 1))
        in_maps.append(m)
    res = run_bass_kernel_spmd(nc, in_maps, core_ids=list(range(NCORES)))
    out = np.concatenate([r["y"].reshape(nseq, SEQ, DM) for r in res.results], axis=0)
    return out.astype(np.float32)
```

```python
import numpy as np
import concourse.bass as bass
import concourse.mybir as mybir
from concourse.bass_utils import run_bass_kernel_spmd
from contextlib import ExitStack

F32 = mybir.dt.float32
BF16 = mybir.dt.bfloat16
I32 = mybir.dt.int32
AF = mybir.ActivationFunctionType
ALU = mybir.AluOpType

NCORES = 8
SEQ = 2048
NT = 16
DM = 1024
DFF = 2816
ALPHA = 2.0 ** 0.25
TWO_PI = float(2 * np.pi)


class Reg:
    _n = 0

    def __init__(self, name=None):
        Reg._n += 1
        self.name = (name or "r") + f"_{Reg._n}"
        self.writers = {}
        self.readers = {}
        self.dma_sem = None
        self.dma_total = 0


class Sched:
    ENG = ["pe", "act", "dve", "pool", "sp"]

    def __init__(self, nc, es):
        self.nc = nc
        self.es = es
        self.sem = {e: es.enter_context(nc.semaphore("s_" + e)) for e in self.ENG}
        self.cnt = {e: 0 for e in self.ENG}
        self.waited = {e: {} for e in self.ENG}
        self.stream = {e: [] for e in self.ENG}
        self.stores = {}
        self.dmakeys = {}

    def dma_sem(self, R):
        if R.dma_sem is None:
            R.dma_sem = self.es.enter_context(self.nc.semaphore("d_" + R.name))
        return R.dma_sem

    def _need(self, e, key, sv, waits):
        if e == "pe" and key == "pe":
            return
        if self.waited[e].get(key, 0) >= sv[1]:
            return
        if key not in waits or waits[key][1] < sv[1]:
            waits[key] = sv

    def op(self, e, fn, reads=(), writes=(), dma=None, store=False):
        waits = {}
        for r in reads:
            for k, sv in r.writers.items():
                self._need(e, k, sv, waits)
        for w in writes:
            for k, sv in w.writers.items():
                self._need(e, k, sv, waits)
            for k, sv in w.readers.items():
                self._need(e, k, sv, waits)
        for k, sv in waits.items():
            self.waited[e][k] = sv[1]
        if dma is None:
            self.cnt[e] += 1
            key = e
            sv = (self.sem[e], self.cnt[e])
            inc = (self.sem[e], 1)
        else:
            s = self.dma_sem(dma)
            dma.dma_total += 16
            key = "d_" + dma.name
            sv = (s, dma.dma_total)
            inc = (s, 16)
            self.dmakeys[key] = sv
            if store:
                self.stores[key] = sv
        for r in reads:
            if key not in r.readers or r.readers[key][1] < sv[1]:
                r.readers[key] = sv
        for w in writes:
            w.writers = {key: sv}
            w.readers = {}
        self.stream[e].append((list(waits.values()), fn, inc))

    def barrier(self):
        for e in self.ENG:
            waits = []
            for f in self.ENG:
                if f != e and self.cnt[f] > self.waited[e].get(f, 0):
                    waits.append((self.sem[f], self.cnt[f]))
                    self.waited[e][f] = self.cnt[f]
            for k, sv in self.dmakeys.items():
                if self.waited[e].get(k, 0) < sv[1]:
                    waits.append(sv)
                    self.waited[e][k] = sv[1]
            self.stream[e].append((waits, None, None))

    def finish(self):
        waits = []
        for k, sv in self.stores.items():
            if self.waited["sp"].get(k, 0) < sv[1]:
                waits.append(sv)
        self.stream["sp"].append((waits, None, None))

    def emit(self):
        nc = self.nc
        with nc.Block() as block:
            def mk(e):
                def body(eng):
                    for waits, fn, inc in self.stream[e]:
                        for s, v in waits:
                            eng.wait_ge(s, v)
                        if fn is not None:
                            fn(eng).then_inc(inc[0], inc[1])
                return body
            block.tensor(mk("pe"))
            block.scalar(mk("act"))
            block.vector(mk("dve"))
            block.gpsimd(mk("pool"))
            block.sync(mk("sp"))


class Buf:
    def __init__(self, ap, name):
        self.ap = ap
        self.r = Reg(name)

    def __getitem__(self, k):
        return self.ap[k]


def bmid(a, k):
    return bass.AP(a.tensor, a.offset, [list(a.ap[0]), [0, k], list(a.ap[1])])


def blast(a, k):
    return bass.AP(a.tensor, a.offset, [list(a.ap[0]), list(a.ap[1]), [0, k]])


C_ID, C_TRI, C_TREV, C_CI, C_M8, C_INVF, C_NG, C_B1, C_B2K, C_V01, C_FB2, C_E6, C_E5, C_ONE, NCFA = \
    0, 128, 256, 384, 386, 394, 426, 554, 558, 560, 1072, 1584, 1585, 1586, 1592
B_ID, B_OV, B_E, B_CAUS, B_ANTI, B_ONES, B_POST, B_B2V, B_TRI, B_TREV, B_CI, NCB = \
    0, 128, 160, 2208, 2720, 3232, 3360, 3392, 3456, 3584, 3712, 3720

ARENA_BYTES = 207 * 1024
O_CFA = 0
O_CB = O_CFA + NCFA * 4
O_ROML = O_CB + NCB * 2
O_A = ((O_ROML + 4096 + 1023) // 1024) * 1024
O_B = O_A + 32768
O_XT = O_B + 32768
O_KSW = O_XT + 32768
O_VSW = O_KSW + 16384
O_KCMP = O_VSW + NT * 4 * 2 * 65 * 2
O_VCMP = O_KCMP + 1024
O_C2 = O_VCMP + 512
O_S2 = O_C2 + 4096
O_SCR = O_S2 + 4096


def build(NSEQ=4, dbg=False, stop=9, skip=(), npair=4, ntile=NT, hgcut=99):
    nc = bass.Bass("TRN2", target_bir_lowering=False)

    def din(name, shape, dt=F32):
        return nc.dram_tensor(name, shape, dt, kind="ExternalInput").ap()

    x = din("x", [NSEQ * SEQ, DM])
    pos = din("pos", [NSEQ, 128, NT], I32)
    wkv = din("wkv", [128, 8, 1536])
    wq = din("wq", [128, 8, 1024])
    wng = din("wng", [128, 8, 48])
    whg = din("whg", [8, 128, 8, 512])
    wga = din("wga", [128, 8, 1024])
    wgb = din("wgb", [128, 8, 1024])
    wuh = din("wuh", [128, 8, 1024])
    wun = din("wun", [128, 8, 1024])
    wo = din("wo", [128, 8, 1024])
    wfg = din("wfg", [128, 8, DFF])
    wfu = din("wfu", [128, 8, DFF])
    wfd = din("wfd", [128, 22, 1024])
    w1 = din("w1", [128, 32, 256])
    w2k = din("w2k", [128, 2, 64])
    w2v = din("w2v", [128, 2, 64])
    cfa_d = din("cfa", [128, NCFA])
    cb_d = din("cb", [128, NCB])
    lbrows = din("lbrows", [128, 2048])
    lnp_d = din("lnp", [128, 4096])
    y = nc.dram_tensor("y", [NSEQ * SEQ, DM], F32, kind="ExternalOutput").ap()
    dbg_out = {}
    if dbg:
        for nm in ("d_aT", "d_bT"):
            dbg_out[nm] = nc.dram_tensor(nm, [128, 8 * SEQ], F32, kind="ExternalOutput").ap()
        dbg_out["d_h1"] = nc.dram_tensor("d_h1", [SEQ, DM], F32, kind="ExternalOutput").ap()

    es = ExitStack()
    with es:
        S = Sched(nc, es)
        arena = es.enter_context(nc.sbuf_tensor("arena", [128, ARENA_BYTES // 2], BF16))
        arena32 = arena.bitcast(F32)
        arenaI = arena.bitcast(I32)
        psf = [es.enter_context(nc.psum_tensor(f"ps{i}", [128, 512], F32)) for i in range(8)]
        PB = [Buf(p[:, :], f"ps{i}") for i, p in enumerate(psf)]
        PBb = [p.bitcast(BF16)[:, :] for p in psf]

        _bufs = {}

        def at(off, n, dt, name):
            key = (off, n, str(dt), name)
            if key not in _bufs:
                _bufs[key] = _at(off, n, dt, name)
            return _bufs[key]

        def _at(off, n, dt, name):
            assert off % 4 == 0 and off + n * (2 if dt == BF16 else 4) <= ARENA_BYTES, (name, off, n)
            if dt == BF16:
                return Buf(arena[:, off // 2: off // 2 + n], name)
            return Buf((arenaI if dt == I32 else arena32)[:, off // 4: off // 4 + n], name)

        class Cur:
            def __init__(self, off, lim=ARENA_BYTES):
                self.off = off
                self.lim = lim

            def take(self, n, dt, name):
                nb = n * (2 if dt == BF16 else 4)
                b = at(self.off, n, dt, name)
                self.off += (nb + 63) // 64 * 64
                assert self.off <= self.lim, (name, self.off, self.lim)
                return b

        def regs(bs):
            return [b.r if isinstance(b, Buf) else b for b in bs]

        def MM(out, lhsT, rhs, start, stop, R, W, skip=False):
            S.op("pe", lambda e: e.matmul(out, lhsT, rhs, start=start, stop=stop, skip_group_check=skip),
                 regs(R), regs(W))

        def TR(out, in_, ident, R, W):
            S.op("pe", lambda e: e.transpose(out, in_, ident), regs(R), regs(W))

        def ACT(out, in_, func, R, W, bias=None, scale=None, accum=None):
            kw = {}
            if bias is not None:
                kw["bias"] = bias
            if scale is not None:
                kw["scale"] = scale
            if accum is not None:
                kw["accum_out"] = accum
            S.op("act", lambda e: e.activation(out=out, in_=in_, func=func, **kw), regs(R), regs(W))

        def TT(out, in0, in1, op, R, W, eng="dve"):
            S.op(eng, lambda e: e.tensor_tensor(out=out, in0=in0, in1=in1, op=op), regs(R), regs(W))

        def TS(out, in0, s1, s2, op0, op1, R, W, eng="dve"):
            if s2 is None:
                S.op(eng, lambda e: e.tensor_scalar(out=out, in0=in0, scalar1=s1, scalar2=None, op0=op0),
                     regs(R), regs(W))
            else:
                S.op(eng, lambda e: e.tensor_scalar(out=out, in0=in0, scalar1=s1, scalar2=s2, op0=op0, op1=op1),
                     regs(R), regs(W))

        def STT(out, in0, scalar, in1, op0, op1, R, W):
            S.op("dve", lambda e: e.scalar_tensor_tensor(out=out, in0=in0, scalar=scalar, in1=in1, op0=op0, op1=op1),
                 regs(R), regs(W))

        def CPV(out, in_, R, W):
            S.op("dve", lambda e: e.tensor_copy(out, in_), regs(R), regs(W))

        def CPA(out, in_, R, W):
            ACT(out, in_, AF.Copy, R, W)

        def RCP(out, in_, R, W):
            S.op("dve", lambda e: e.reciprocal(out, in_), regs(R), regs(W))

        def MSET(out, val, W, eng="dve"):
            S.op(eng, lambda e: e.memset(out, val), [], regs(W))

        def DMA(q, out, in_, R, W, semb, store=False):
            S.op(q, lambda e: e.dma_start(out=out, in_=in_), regs(R), regs(W), dma=semb.r, store=store)

        cfa = at(O_CFA, NCFA, F32, "cfa")
        cb = at(O_CB, NCB, BF16, "cb")
        roml = at(O_ROML, 1024, F32, "roml")
        bufA = at(O_A, 8 * SEQ, BF16, "A")
        bufB = at(O_B, 8 * SEQ, BF16, "B")
        xT = at(O_XT, 8 * SEQ, BF16, "xT")
        ksw = at(O_KSW, 4 * SEQ, BF16, "ksw")
        vsw = at(O_VSW, NT * 4 * 2 * 65, BF16, "vsw")
        kcmp = at(O_KCMP, 4 * 128, BF16, "kcmp")
        vcmp = at(O_VCMP, 4 * 64, BF16, "vcmp")
        c2 = at(O_C2, NT * 64, F32, "c2")
        s2 = at(O_S2, NT * 64, F32, "s2")
        aTv = bufB.ap.rearrange("p (c n) -> p c n", c=8)
        bTv = bufA.ap.rearrange("p (c n) -> p c n", c=8)
        xTv = xT.ap.rearrange("p (c n) -> p c n", c=8)
        kswv = ksw.ap.rearrange("p (h n) -> p h n", h=4)
        vswv = vsw.ap.rearrange("p (t h b d) -> p t h b d", t=NT, h=4, b=2)
        kcmpv = kcmp.ap.rearrange("p (h n) -> p h n", h=4)
        vcmpv = vcmp.ap.rearrange("p (h d) -> p h d", h=4)
        c2v = c2.ap.rearrange("p (t d) -> p t d", t=NT)
        s2v = s2.ap.rearrange("p (t d) -> p t d", t=NT)
        identf = cfa[:, C_ID:C_ID + 128]
        identb = cb[:, B_ID:B_ID + 128]

        DMA("sp", cfa.ap, cfa_d, [], [cfa], cfa)
        DMA("pool", cb.ap, cb_d, [], [cb], cb)
        ci = Cur(O_SCR)
        lbt = ci.take(2048, F32, "lbt")
        DMA("sp", lbt.ap, lbrows, [], [lbt], lbt)
        TT(lbt[:, 0:1024], lbt[:, 0:1024], lbt[:, 1024:2048], ALU.subtract, [lbt], [lbt])
        ACT(roml.ap, lbt[:, 0:1024], AF.Exp, [lbt], [roml])
        TS(roml.ap, roml.ap, 1.0, None, ALU.add, None, [roml], [roml])

        def rope(src_ps, srcbuf, nh, tile, t1, t2, dst_list):
            sv = src_ps.rearrange("p (h t d) -> p h t d", h=nh, t=2)
            t1v = t1.ap[:, 0:nh * 64].rearrange("p (h d) -> p h d", h=nh)
            t2v = t2.ap[:, 0:nh * 64].rearrange("p (h t d) -> p h t d", h=nh, t=2)
            TT(t1v, src_ps.rearrange("p (h d) -> p h d", h=nh), bmid(c2v[:, tile, :], nh), ALU.mult,
               [srcbuf, c2], [t1])
            TT(t2v[:, :, 0, :], sv[:, :, 1, :], bmid(s2v[:, tile, 0:32], nh), ALU.mult, [srcbuf, s2], [t2])
            TT(t2v[:, :, 1, :], sv[:, :, 0, :], bmid(s2v[:, tile, 32:64], nh), ALU.mult, [srcbuf, s2, t2], [t2])
            t2f = t2.ap[:, 0:nh * 64].rearrange("p (h d) -> p h d", h=nh)
            for dap, dbuf in dst_list:
                TT(dap, t1v, t2f, ALU.add, [t1, t2], [dbuf])

        for s in range(NSEQ):
            tok0 = s * SEQ
            S.barrier()
            ct = at(O_A, 4 * SEQ, BF16, "ct")
            ctv = ct.ap.rearrange("p (h n) -> p h n", h=4)
            w1s = at(O_A + 16384, 32 * 256, BF16, "w1s")
            w1v_ = w1s.ap.rearrange("p (l n) -> p l n", l=32)
            wkvs = at(O_B, 8 * 1536, BF16, "wkvs")
            wkvv = wkvs.ap.rearrange("p (c n) -> p c n", c=8)
            MSET(vsw.ap, 1.0, [vsw])
            DMA("pool", wkvv, wkv, [], [wkvs], wkvs)
            DMA("pool", w1v_, w1, [], [w1s], w1s)
            cu = Cur(O_SCR)
            w2ks = cu.take(128, BF16, "w2ks")
            w2vs = cu.take(128, BF16, "w2vs")
            DMA("pool", w2ks.ap.rearrange("p (h d) -> p h d", h=2), w2k, [], [w2ks], w2ks)
            DMA("pool", w2vs.ap.rearrange("p (h d) -> p h d", h=2), w2v, [], [w2vs], w2vs)
            posi = cu.take(NT, I32, "posi")
            posf = cu.take(NT, F32, "posf")
            ang = cu.take(NT * 32, F32, "ang")
            ang2 = cu.take(NT * 32, F32, "ang2")
            kq = cu.take(NT * 32, F32, "kq")
            kqi = cu.take(NT * 32, I32, "kqi")
            DMA("sp", posi.ap, pos[s], [], [posi], posi)
            CPV(posf.ap, posi.ap, [posi], [posf])
            angv = ang.ap.rearrange("p (t j) -> p t j", t=NT)
            TT(angv, blast(posf.ap, 32), bmid(cfa[:, C_INVF:C_INVF + 32], NT), ALU.mult, [posf, cfa], [ang])
            TS(ang2.ap, ang.ap, float(np.pi / 2), None, ALU.add, None, [ang], [ang2])
            for src, which in ((ang, 0), (ang2, 1)):
                TS(kq.ap, src.ap, 1.0 / TWO_PI, None, ALU.mult, None, [src], [kq])
                CPV(kqi.ap, kq.ap, [kq], [kqi])
                CPV(kq.ap, kqi.ap, [kqi], [kq])
                STT(src.ap, kq.ap, -TWO_PI, src.ap, ALU.mult, ALU.add, [kq, src], [src])
                TS(src.ap, src.ap, 3.1415925, -3.1415925, ALU.min, ALU.max, [src], [src])
                srcv = src.ap.rearrange("p (t j) -> p t j", t=NT)
                if which == 0:
                    ACT(s2v[:, :, 32:64], srcv, AF.Sin, [src], [s2])
                    TS(s2v[:, :, 0:32], s2v[:, :, 32:64], -1.0, None, ALU.mult, None, [s2], [s2])
                else:
                    ACT(c2v[:, :, 0:32], srcv, AF.Sin, [src], [c2])
                    CPV(c2v[:, :, 32:64], c2v[:, :, 0:32], [c2], [c2])
            xin = [cu.take(1024, F32, f"xin{i}") for i in range(2)]
            t1 = cu.take(256, F32, "t1")
            t2 = cu.take(256, F32, "t2")
            krc = cu.take(256, BF16, "krc")
            krcv = krc.ap.rearrange("p (h d) -> p h d", h=4)
            for i in range(ntile if 1 in skip else NT):
                xb = xin[i % 2]
                DMA("sp", xb.ap, x[tok0 + i * 128: tok0 + (i + 1) * 128, :], [], [xb], xb)
                for c in range(8):
                    pb = PB[c // 4]
                    TR(pb[:, (c % 4) * 128:(c % 4 + 1) * 128], xb[:, c * 128:(c + 1) * 128], identf, [xb, cfa], [pb])
                CPA(xTv[:, 0:4, i * 128:(i + 1) * 128], PB[0].ap.rearrange("p (c n) -> p c n", c=4), [PB[0]], [xT])
                CPV(xTv[:, 4:8, i * 128:(i + 1) * 128], PB[1].ap.rearrange("p (c n) -> p c n", c=4), [PB[1]], [xT])
                for hk in range(4):
                    pb = PB[2 + hk % 2]
                    for c in range(8):
                        MM(pb[:, 0:384], xTv[:, c, i * 128:(i + 1) * 128], wkvv[:, c, hk * 384:(hk + 1) * 384],
                           c == 0, c == 7, [xT, wkvs], [pb])
                    rope(pb[:, 0:192], pb, 3, i, t1, t2, [(krcv[:, 0:3, :], krc)])
                    CPA(krc[:, 192:256], pb[:, 192:256], [pb], [krc])
                    CPA(vswv[:, i, hk, :, 0:64], pb[:, 256:384].rearrange("p (b d) -> p b d", b=2), [pb], [vsw])
                    TR(PBb[7][:, 0:128], krc[:, 0:128], identb, [krc, cb], [PB[7]])
                    TR(PBb[7][:, 128:256], krc[:, 128:256], identb, [krc, cb], [PB[7]])
                    CPV(kswv[:, hk, i * 128:(i + 1) * 128], PBb[7][:, 0:128], [PB[7]], [ksw])
                    CPA(ctv[:, hk, i * 128:(i + 1) * 128], PBb[7][:, 128:256], [PB[7]], [ct])
            c1 = cu.take(4, F32, "c1")
            hdn = cu.take(4 * 128, BF16, "hdn")
            hdnv = hdn.ap.rearrange("p (a n) -> p a n", a=4)
            for kv in range(2):
                pb = PB[kv]
                pr = slice(64 * kv, 64 * kv + 64)
                for half in range(2):
                    col = kv * 2 + half
                    for l in range(32):
                        MM(pb[:, col:col + 1], w1v_[pr, l, half * 128:(half + 1) * 128],
                           cb[pr, B_POST + l:B_POST + l + 1], l == 0, l == 31, [w1s, cb], [pb])
                TT(c1[:, 2 * kv:2 * kv + 2], pb[:, 2 * kv:2 * kv + 2], cfa[:, C_B1 + 2 * kv:C_B1 + 2 * kv + 2], ALU.add,
                   [pb, cfa], [c1])
            for hk in range(0 if 1 in skip else 4):
                for kv in range(2):
                    pr = slice(64 * kv, 64 * kv + 64)
                    for half in range(2):
                        col = kv * 2 + half
                        pb = PB[1 + (col % 2)]
                        for l in range(32):
                            MM(pb[:, 0:127], w1v_[pr, l, half * 128:(half + 1) * 128],
                               ctv[pr, hk, l:l + 2017:16], l == 0, l == 31, [w1s, ct], [pb])
                        ACT(hdnv[:, col, 0:127], pb[:, 0:127], AF.Silu, [pb, c1], [hdn], bias=c1[:, col:col + 1])
                pb = PB[3]
                w2kv = w2ks.ap.rearrange("p (h d) -> p h d", h=2)
                w2vv = w2vs.ap.rearrange("p (h d) -> p h d", h=2)
                for half in range(2):
                    MM(pb[0:64, 0:127], w2kv[:, half, :], hdnv[:, half, 0:127], half == 0, half == 1, [w2ks, hdn], [pb])
                ACT(kcmpv[0:64, hk, 0:127], pb[0:64, 0:127], AF.Identity, [pb, cfa], [kcmp],
                    bias=cfa[0:64, C_B2K:C_B2K + 1])
                pb = PB[4]
                for half in range(2):
                    MM(pb[0:127, 0:64], hdnv[:, 2 + half, 0:127], w2vv[:, half, :], half == 0, False, [w2vs, hdn], [pb])
                MM(pb[0:127, 0:64], cb[0:1, B_ONES:B_ONES + 127], cb[0:1, B_B2V:B_B2V + 64], False, True, [cb], [pb])
                CPV(vcmpv[0:127, hk, :], pb[0:127, 0:64], [pb], [vcmp])

            if stop < 2:
                continue
            S.barrier()
            cu = Cur(O_SCR)
            wqs = cu.take(8 * 1024, BF16, "wqs")
            wqv = wqs.ap.rearrange("p (c n) -> p c n", c=8)
            wngs = cu.take(8 * 48, BF16, "wngs")
            wngv = wngs.ap.rearrange("p (c n) -> p c n", c=8)
            DMA("pool", wqv, wq, [], [wqs], wqs)
            DMA("pool", wngv, wng, [], [wngs], wngs)
            t1 = cu.take(256, F32, "t1a")
            t2 = cu.take(256, F32, "t2a")
            qr2 = cu.take(512, BF16, "qr2")
            qr2v = qr2.ap.rearrange("p (g b d) -> p g b d", g=4, b=2)
            qt2 = cu.take(512, BF16, "qt2")
            qt2v = qt2.ap.rearrange("p (g n) -> p g n", g=4)
            ec = cu.take(512, F32, "ec")
            ecv = ec.ap.rearrange("p (g n) -> p g n", g=4)
            pcb = cu.take(512, BF16, "pcb")
            pcbv = pcb.ap.rearrange("p (g n) -> p g n", g=4)
            pct = cu.take(512, BF16, "pct")
            pctv = pct.ap.rearrange("p (g n) -> p g n", g=4)
            st = cu.take(64, F32, "st")
            sc = cu.take(32, F32, "sc")
            nsl = cu.take(32, BF16, "nsl")
            rb = cu.take(512, BF16, "rb")
            rbv = rb.ap.rearrange("p (g n) -> p g n", g=4)
            pt = [cu.take(512, BF16, f"pt{i}") for i in range(2)]
            sg = cu.take(48, F32, "sg")
            oc = cu.take(256, F32, "oc")
            fac = cu.take(8, F32, "fac")
            btile = cu.take(256, BF16, "btile")
            acc = cu.take(256, F32, "acc")
            tmp = cu.take(256, F32, "tmpc")
            Ev = cb[0:32, B_E:B_E + 2048].rearrange("p (j n) -> p j n", j=16)
            qt2s = [qt2, cu.take(512, BF16, "qt2b")]
            rbs = [rb, cu.take(512, BF16, "rbb")]
            ocs = [oc, cu.take(256, F32, "ocb")]
            sgs = [sg, cu.take(48, F32, "sgb")]
            iters = [(i, hk) for i in range(0 if 2 in skip else NT) for hk in range(4)]

            def att_front(n):
                i, hk = iters[n]
                p = n % 2
                tsl = slice(i * 128, (i + 1) * 128)
                ncv = min(127, 8 * i + 7)
                qt2_, rb_, oc_, sg_ = qt2s[p], rbs[p], ocs[p], sgs[i % 2]
                qt2v_ = qt2_.ap.rearrange("p (g n) -> p g n", g=4)
                rbv_ = rb_.ap.rearrange("p (g n) -> p g n", g=4)
                if hk == 0:
                    pb = PB[0]
                    for c in range(8):
                        MM(pb[:, 0:48], xTv[:, c, tsl], wngv[:, c, :], c == 0, c == 7, [xT, wngs], [pb])
                    ACT(sg_.ap, pb[:, 0:48], AF.Exp, [pb], [sg_], scale=-1.0)
                    TS(sg_.ap, sg_.ap, 1.0, None, ALU.add, None, [sg_], [sg_])
                    RCP(sg_.ap, sg_.ap, [sg_], [sg_])
                pb = PB[1]
                for c in range(8):
                    MM(pb[:, 0:256], xTv[:, c, tsl], wqv[:, c, hk * 256:(hk + 1) * 256], c == 0, c == 7,
                       [xT, wqs], [pb])
                rope(pb[:, 0:256], pb, 4, i, t1, t2, [(qr2v[:, :, 0, :], qr2), (qr2v[:, :, 1, :], qr2)])
                yield
                for g in range(4):
                    TR(PBb[7][:, g * 128:(g + 1) * 128], qr2[:, g * 128:(g + 1) * 128], identb, [qr2, cb], [PB[7]])
                CPA(qt2_.ap, PBb[7][:, 0:512], [PB[7]], [qt2_])
                yield
                ps = PB[0]
                psv = ps.ap.rearrange("p (g n) -> p g n", g=4)
                for g in range(4):
                    MM(psv[:, g, 0:ncv], qt2v_[0:64, g, :], kcmpv[0:64, hk, 0:ncv], True, True, [qt2_, kcmp], [ps])
                j0 = 1 if i == 0 else 0
                lo = ncv - (8 - j0)
                TT(psv[:, :, lo:ncv], psv[:, :, lo:ncv], bmid(cfa[:, C_M8 + j0:C_M8 + 8], 4), ALU.add,
                   [ps, cfa], [ps])
                S.op("dve", lambda e, o=st[:, 0:4], a=psv[:, :, 0:ncv]: e.tensor_reduce(
                    out=o, in_=a, axis=mybir.AxisListType.X, op=ALU.max), [ps.r], [st.r])
                TS(st[:, 4:8], st[:, 0:4], -1e4, -0.125, ALU.max, ALU.mult, [st], [st])
                MSET(st[:, 8:12], 0.0, [st])
                for g in range(4):
                    ACT(ecv[:, g, 0:ncv], psv[:, g, 0:ncv], AF.Exp, [ps, st], [ec, st],
                        bias=st[:, 4 + g:5 + g], scale=0.125, accum=st[:, 8 + g:9 + g])
                TS(st[:, 12:16], st[:, 8:12], 1e-30, None, ALU.max, None, [st], [st])
                RCP(st[:, 12:16], st[:, 12:16], [st], [st])
                TT(pcbv[:, :, 0:ncv], ecv[:, :, 0:ncv], blast(st[:, 12:16], ncv), ALU.mult, [ec, st], [pcb])
                yield
                for g in range(4):
                    TR(PBb[7][0:ncv, g * 128:(g + 1) * 128], pcbv[:, g, 0:ncv], identb, [pcb, cb], [PB[7]])
                CPA(pctv[0:ncv, :, :], PBb[7][0:ncv, 0:512].rearrange("p (g n) -> p g n", g=4), [PB[7]], [pct])
                yield
                po = PB[6]
                for g in range(4):
                    MM(po[:, 256:288], pctv[0:ncv, g, :], cb[0:ncv, B_OV:B_OV + 32], g == 0, g == 3,
                       [pct, cb], [po], skip=True)
                for g in range(4):
                    MM(po[:, g * 64:(g + 1) * 64], pctv[0:ncv, g, :], vcmpv[0:ncv, hk, :], False, True,
                       [pct, vcmp], [po], skip=True)
                TT(sc.ap, po[:, 256:288], cfa[:, C_V01 + i * 32:C_V01 + (i + 1) * 32], ALU.mult, [po, cfa], [sc])
                TT(sc.ap, sc.ap, cfa[:, C_FB2 + i * 32:C_FB2 + (i + 1) * 32], ALU.add, [sc, cfa], [sc])
                S.op("dve", lambda e, o=st[:, 16:24], a=sc.ap: e.max(out=o, in_=a), [sc.r], [st.r])
                TS(sc.ap, sc.ap, st[:, 23:24], 30000.0, ALU.is_ge, ALU.mult, [sc, st], [sc])
                TS(nsl.ap, sc.ap, -30000.0, None, ALU.add, None, [sc], [nsl])
                ocv = oc_.ap.rearrange("p (g d) -> p g d", g=4)
                TT(ocv, po[:, 0:256].rearrange("p (g d) -> p g d", g=4), blast(sg_[:, hk * 4:hk * 4 + 4], 64),
                   ALU.mult, [po, sg_], [oc_])
                yield
                TR(PBb[7][0:32, 0:128], nsl.ap, identb, [nsl, cb], [PB[7]])
                CPA(rbv_[0:32, :, :], bmid(PBb[7][0:32, 0:128], 4), [PB[7]], [rb_])

            ptc = [0]

            def att_back(n, gen_next):
                i, hk = iters[n]
                p = n % 2
                tsl = slice(i * 128, (i + 1) * 128)
                qt2_, rb_, oc_, sg_ = qt2s[p], rbs[p], ocs[p], sgs[i % 2]
                pos_ = PB[4]
                pow_ = PB[5]
                jlo = max(0, i - 4)
                items = [("s", j) for j in range(i + 1)] + [("w", j) for j in range(jlo, i + 1)]

                def emit_S(m, base):
                    kind, j = items[m]
                    ps = PB[2 + ((base + m) % 2)]
                    if kind == "s":
                        MM(ps.ap, kswv[0:64, hk, j * 128:(j + 1) * 128], qt2_[0:64, :], True, False, [ksw, qt2_], [ps])
                        MM(ps.ap, Ev[:, j, :], rb_[0:32, :], False, j != i, [cb, rb_], [ps])
                        if j == i:
                            MM(ps.ap, identb, cb[:, B_CAUS:B_CAUS + 512], False, True, [cb], [ps])
                    else:
                        msk = None
                        if j == i:
                            msk = B_CAUS
                        elif j == i - 4:
                            msk = B_ANTI
                        MM(ps.ap, kswv[64:128, hk, j * 128:(j + 1) * 128], qt2_[64:128, :], True, msk is None,
                           [ksw, qt2_], [ps])
                        if msk is not None:
                            MM(ps.ap, identb, cb[:, msk:msk + 512], False, True, [cb], [ps])

                def emit_EXP_PV(m, base):
                    kind, j = items[m]
                    ps = PB[2 + ((base + m) % 2)]
                    ptb = pt[(base + m) % 2]
                    ACT(ptb.ap, ps.ap, AF.Exp, [ps], [ptb], scale=0.125)
                    if kind == "s":
                        for g in range(4):
                            MM(pos_[:, g * 65:(g + 1) * 65], ptb[:, g * 128:(g + 1) * 128], vswv[:, j, hk, 0, :],
                               (j == 0 and g == 0), j == i, [ptb, vsw], [pos_], skip=True)
                    else:
                        for g in range(4):
                            MM(pow_[:, g * 65:(g + 1) * 65], ptb[:, g * 128:(g + 1) * 128], vswv[:, j, hk, 1, :],
                               (j == jlo and g == 0), j == i, [ptb, vsw], [pow_], skip=True)

                base = ptc[0]
                stride = max(1, len(items) // 6)
                emit_S(0, base)
                for m in range(len(items)):
                    if m + 1 < len(items):
                        emit_S(m + 1, base)
                    emit_EXP_PV(m, base)
                    if gen_next is not None and m % stride == stride - 1:
                        next(gen_next, None)
                ptc[0] += len(items)
                if gen_next is not None:
                    for _ in gen_next:
                        pass
                accv = acc.ap.rearrange("p (g d) -> p g d", g=4)
                tmpv = tmp.ap.rearrange("p (g d) -> p g d", g=4)
                for br, pacc in ((1, pos_), (2, pow_)):
                    pv = pacc[:, 0:260].rearrange("p (g d) -> p g d", g=4)
                    TS(fac[:, 0:4], pv[:, :, 64], 1e-30, None, ALU.max, None, [pacc], [fac])
                    RCP(fac[:, 0:4], fac[:, 0:4], [fac], [fac])
                    TT(fac[:, 4:8], fac[:, 0:4], sg_[:, br * 16 + hk * 4:br * 16 + hk * 4 + 4], ALU.mult,
                       [fac, sg_], [fac])
                    TT(tmpv, pv[:, :, 0:64], blast(fac[:, 4:8], 64), ALU.mult, [pacc, fac], [tmp])
                    if br == 1:
                        TT(acc.ap, tmp.ap, oc_.ap, ALU.add, [tmp, oc_], [acc])
                    else:
                        TT(btile.ap, tmp.ap, acc.ap, ALU.add, [tmp, acc], [btile])
                for hh in range(2):
                    TR(PBb[7][:, hh * 128:(hh + 1) * 128], btile[:, hh * 128:(hh + 1) * 128], identb,
                       [btile, cb], [PB[7]])
                CPV(bTv[:, hk * 2:hk * 2 + 2, tsl], PBb[7][:, 0:256].rearrange("p (c n) -> p c n", c=2),
                    [PB[7]], [bufA])

            if iters:
                for _ in att_front(0):
                    pass
                for n in range(len(iters)):
                    att_back(n, att_front(n + 1) if n + 1 < len(iters) else None)

            if stop < 3:
                continue
            S.barrier()
            cu = Cur(O_SCR)
            wh = [cu.take(8 * 512, BF16, f"wh{k}") for k in range(4)]

            def load_wh(h):
                b = wh[h % 4]
                DMA("pool", b.ap.rearrange("p (c n) -> p c n", c=8), whg[h], [], [b], b)

            def f32t(nm):
                return [cu.take(128, F32, f"{nm}{k}") for k in range(2)]

            def b16t(nm):
                return [cu.take(128, BF16, f"{nm}{k}") for k in range(2)]

            e1, kk, logf, eb, gs, sq = f32t("e1"), f32t("kk"), f32t("logf"), f32t("eb"), f32t("gs"), f32t("sq")
            qd, ki, ke, vv, attn, qd0, qd1, kit, ab = (b16t("qd"), b16t("ki"), b16t("ke"), b16t("vv"),
                                                       b16t("attn"), b16t("qd0"), b16t("qd1"), b16t("kit"), b16t("ab"))
            dec = f32t("dec")
            lhi, llo = b16t("lhi"), b16t("llo")
            sst = f32t("sst")
            Sf = f32t("Sf")
            Sb0, Sb1 = b16t("Sb0"), b16t("Sb1")
            for k in range(2):
                MSET(qd0[k].ap, 0.0, [qd0[k]])
                MSET(qd1[k].ap, 0.0, [qd1[k]])
            load_wh(0)
            load_wh(1)
            tri = cfa[:, C_TRI:C_TRI + 128]
            trev = cfa[:, C_TREV:C_TREV + 128]
            for pair in range(npair):
                if pair < 3:
                    load_wh(2 * pair + 2)
                    load_wh(2 * pair + 3)
                for k in range(2):
                    MSET(Sf[k].ap, 0.0, [Sf[k]])
                    MSET(Sb0[k].ap, 0.0, [Sb0[k]])
                def hg_proj(i_, k_):
                    h_ = 2 * pair + k_
                    whv_ = wh[h_ % 4].ap.rearrange("p (c n) -> p c n", c=8)
                    for c in range(8):
                        MM(PB[k_].ap, xTv[:, c, i_ * 128:(i_ + 1) * 128], whv_[:, c, :], c == 0, c == 7,
                           [xT, wh[h_ % 4]], [PB[k_]])

                hg_proj(0, 0)
                for i in range(ntile):
                    tsl = slice(i * 128, (i + 1) * 128)
                    for k in range(2):
                        h = 2 * pair + k
                        pp = PB[k]
                        if k == 0:
                            hg_proj(i, 1)
                        elif i + 1 < ntile:
                            hg_proj(i + 1, 0)
                        if hgcut < 1:
                            continue
                        rm = roml[:, h * 128:(h + 1) * 128]
                        ACT(e1[k].ap, pp[:, 128:256], AF.Exp, [pp], [e1[k]])
                        STT(e1[k].ap, e1[k].ap, 1.0, rm, ALU.add, ALU.mult, [e1[k], roml], [e1[k]])
                        RCP(kk[k].ap, e1[k].ap, [e1[k]], [kk[k]])
                        if hgcut < 2:
                            continue
                        ACT(logf[k].ap, kk[k].ap, AF.Ln, [kk[k], cfa], [logf[k]], scale=-1.0, bias=cfa[:, C_ONE:C_ONE + 1])
                        if hgcut < 3:
                            continue
                        pc_ = PB[2 + k]
                        CPA(lhi[k].ap, logf[k].ap, [logf[k]], [lhi[k]])
                        TT(llo[k].ap, logf[k].ap, lhi[k].ap, ALU.subtract, [logf[k], lhi[k]], [llo[k]])
                        if hgcut < 4:
                            continue
                        trib = cb[:, B_TRI:B_TRI + 128]
                        trevb = cb[:, B_TREV:B_TREV + 128]
                        cib = cb[:, B_CI:B_CI + 2]
                        MM(pc_[:, 0:128], trib, lhi[k].ap, True, False, [cb, lhi[k]], [pc_])
                        MM(pc_[:, 0:128], trib, llo[k].ap, False, True, [cb, llo[k]], [pc_])
                        MM(pc_[:, 128:256], trevb, lhi[k].ap, True, False, [cb, lhi[k]], [pc_])
                        MM(pc_[:, 128:256], trevb, llo[k].ap, False, True, [cb, llo[k]], [pc_])
                        if hgcut < 5:
                            continue
                        MM(pc_[:, 256:258], lhi[k].ap, cib, True, False, [cb, lhi[k]], [pc_])
                        MM(pc_[:, 256:258], llo[k].ap, cib, False, True, [cb, llo[k]], [pc_])
                        if hgcut < 6:
                            continue
                        ACT(eb[k].ap, pc_[:, 0:128], AF.Exp, [pc_], [eb[k]])
                        TT(qd[k].ap, pp[:, 0:128], eb[k].ap, ALU.mult, [pp, eb[k]], [qd[k]])
                        ACT(eb[k].ap, pc_[:, 0:128], AF.Exp, [pc_, qd[k]], [eb[k]], scale=-1.0)
                        TT(ki[k].ap, kk[k].ap, eb[k].ap, ALU.mult, [kk[k], eb[k]], [ki[k]])
                        ACT(eb[k].ap, pc_[:, 128:256], AF.Exp, [pc_, ki[k]], [eb[k]])
                        TT(ke[k].ap, kk[k].ap, eb[k].ap, ALU.mult, [kk[k], eb[k]], [ke[k]])
                        if hgcut < 7:
                            continue
                        ACT(dec[k][:, 0:2], pc_[:, 256:258], AF.Exp, [pc_], [dec[k]])
                        CPA(vv[k].ap, pp[:, 256:384], [pp], [vv[k]])
                        if hgcut < 8:
                            continue
                        ACT(gs[k].ap, pp[:, 384:512], AF.Exp, [pp], [gs[k]], scale=-1.0)
                        TS(gs[k].ap, gs[k].ap, 1.0, None, ALU.add, None, [gs[k]], [gs[k]])
                        RCP(gs[k].ap, gs[k].ap, [gs[k]], [gs[k]])
                        TT(gs[k].ap, gs[k].ap, cfa[:, C_NG:C_NG + 128], ALU.mult, [gs[k], cfa], [gs[k]])
                        TT(gs[k].ap, pp[:, 384:512], gs[k].ap, ALU.mult, [gs[k], pp], [gs[k]])
                        if hgcut < 9:
                            continue
                        pt_ = PB[7]
                        TR(PBb[7][:, 0:128], qd[k].ap, identb, [qd[k], cb], [pt_])
                        TR(PBb[7][:, 128:256], ki[k].ap, identb, [ki[k], cb], [pt_])
                        CPA(qd0[k][:, 0:64], PBb[7][:, 0:64], [pt_], [qd0[k]])
                        CPV(qd1[k][:, 64:128], PBb[7][:, 64:128], [pt_], [qd1[k]])
                        CPA(kit[k].ap, PBb[7][:, 128:256], [pt_], [kit[k]])
                        if hgcut < 10:
                            continue
                        pa = PB[4 + k]
                        MM(pa[:, 0:128], kit[k].ap, qd0[k].ap, True, False, [kit[k], qd0[k]], [pa])
                        MM(pa[:, 0:128], kit[k].ap, qd1[k].ap, False, True, [kit[k], qd1[k]], [pa])
                        TT(attn[k].ap, pa[:, 0:128], tri, ALU.mult, [pa, cfa], [attn[k]])
                        if hgcut < 11:
                            continue
                        pu = PB[6]
                        MM(pu[:, 0:128], ke[k][0:64, :], vv[k][0:64, :], True, True, [ke[k], vv[k]], [pu])
                        MM(pc_[:, 384:512], ke[k][64:128, :], vv[k][64:128, :], True, True, [ke[k], vv[k]], [pc_])
                        if hgcut < 11.2:
                            continue
                        TS(Sf[k].ap, Sf[k].ap, dec[k][:, 0:1], None, ALU.mult, None, [Sf[k], dec[k]], [Sf[k]])
                        TT(Sf[k].ap, pu[:, 0:128], Sf[k].ap, ALU.add, [Sf[k], pu], [Sf[k]])
                        CPA(Sb1[k].ap, Sf[k].ap, [Sf[k]], [Sb1[k]])
                        if hgcut < 11.4:
                            continue
                        MM(pa[:, 128:256], attn[k].ap, vv[k].ap, True, False, [attn[k], vv[k]], [pa])
                        MM(pa[:, 128:256], qd0[k].ap, Sb0[k].ap, False, False, [qd0[k], Sb0[k]], [pa])
                        MM(pa[:, 128:256], qd1[k].ap, Sb1[k].ap, False, True, [qd1[k], Sb1[k]], [pa])
                        if hgcut < 11.6:
                            continue
                        TS(Sf[k].ap, Sf[k].ap, dec[k][:, 1:2], None, ALU.mult, None, [Sf[k], dec[k]], [Sf[k]])
                        TT(Sf[k].ap, pc_[:, 384:512], Sf[k].ap, ALU.add, [Sf[k], pc_], [Sf[k]])
                        CPA(Sb0[k].ap, Sf[k].ap, [Sf[k]], [Sb0[k]])
                        if hgcut < 12:
                            continue
                        MSET(sst[k][:, 0:1], 0.0, [sst[k]])
                        ACT(sq[k].ap, pa[:, 128:256], AF.Square, [pa, sst[k]], [sq[k], sst[k]], accum=sst[k][:, 0:1])
                        if hgcut < 13:
                            continue
                        ACT(sst[k][:, 1:2], sst[k][:, 0:1], AF.Ln, [sst[k], cfa], [sst[k]], scale=1.0 / 128, bias=cfa[:, C_E6:C_E6 + 1])
                        ACT(sst[k][:, 2:3], sst[k][:, 1:2], AF.Exp, [sst[k]], [sst[k]], scale=-0.5)
                        if hgcut < 14:
                            continue
                        STT(ab[k].ap, pa[:, 128:256], sst[k][:, 2:3], gs[k].ap, ALU.mult, ALU.mult,
                            [pa, sst[k], gs[k]], [ab[k]])
                        TR(PBb[7][:, 256:384], ab[k].ap, identb, [ab[k], cb], [pt_])
                        CPV(aTv[:, h, tsl], PBb[7][:, 256:384], [pt_], [bufB])

            if dbg and s == 0:
                S.barrier()
                for nm, b in (("d_aT", bufB), ("d_bT", bufA)):
                    dtmp = at(O_XT, 8192, F32, "dtmp" + nm)
                    for q in range(2):
                        CPV(dtmp.ap, b[:, q * 8192:(q + 1) * 8192], [b], [dtmp])
                        DMA("sp", dbg_out[nm][:, q * 8192:(q + 1) * 8192], dtmp.ap, [dtmp], [], dtmp, store=True)

            if stop < 4:
                continue
            S.barrier()
            cu = Cur(O_XT)
            xtg = cu.take(8 * 512, BF16, "xtg")
            xtgv = xtg.ap.rearrange("p (c n) -> p c n", c=8)
            hx = [cu.take(1024, F32, f"hx{k}") for k in range(4)]
            h1t = cu.take(8 * 512, BF16, "h1t")
            h1tv = h1t.ap.rearrange("p (c n) -> p c n", c=8)
            mgt = cu.take(8 * 512, BF16, "mgt")
            mgtv = mgt.ap.rearrange("p (c n) -> p c n", c=8)
            actt = cu.take(22 * 512, BF16, "actt")
            acttv = actt.ap.rearrange("p (c n) -> p c n", c=22)
            ring = [cu.take(4096, BF16, f"ring{k}") for k in range(4)]
            sga = cu.take(512, F32, "sga")
            sgb = cu.take(512, F32, "sgb")
            m1 = cu.take(512, F32, "m1")
            m2 = cu.take(512, F32, "m2")
            hb = cu.take(1024, BF16, "hb")
            lnp = cu.take(4096, F32, "lnp")
            lst = cu.take(16, F32, "lst")
            junk = m1
            DMA("sp", lnp.ap, lnp_d, [], [lnp], lnp)
            rc = [0]

            def wload(src3d, ncol_total, c0, ncols, kdim=8):
                b = ring[rc[0] % 4]
                rc[0] += 1
                v = b.ap[:, 0:kdim * ncols].rearrange("p (c n) -> p c n", c=kdim)
                DMA("pool", v, src3d[:, :, c0:c0 + ncols], [], [b], b)
                return b, v

            def layer_norm(src, gcol, bcol, dst_f32, dst_buf):
                MSET(lst[:, 8:12], 0.0, [lst])
                ACT(junk[:, 0:512], src[:, 0:512], AF.Identity, [src, lst], [junk, lst], accum=lst[:, 8:9])
                ACT(junk[:, 0:512], src[:, 512:1024], AF.Identity, [src, lst], [junk, lst], accum=lst[:, 10:11])
                ACT(junk[:, 0:512], src[:, 0:512], AF.Square, [src, lst], [junk, lst], accum=lst[:, 9:10])
                ACT(junk[:, 0:512], src[:, 512:1024], AF.Square, [src, lst], [junk, lst], accum=lst[:, 11:12])
                TT(lst[:, 0:2], lst[:, 8:10], lst[:, 10:12], ALU.add, [lst], [lst])
                TS(lst[:, 2:4], lst[:, 0:2], 1.0 / 1024, None, ALU.mult, None, [lst], [lst])
                TT(lst[:, 4:5], lst[:, 2:3], lst[:, 2:3], ALU.mult, [lst], [lst])
                TT(lst[:, 5:6], lst[:, 3:4], lst[:, 4:5], ALU.subtract, [lst], [lst])
                ACT(lst[:, 6:7], lst[:, 5:6], AF.Ln, [lst, cfa], [lst], bias=cfa[:, C_E5:C_E5 + 1])
                ACT(lst[:, 7:8], lst[:, 6:7], AF.Exp, [lst], [lst], scale=-0.5)
                TS(src.ap, src.ap, lst[:, 2:3], lst[:, 7:8], ALU.subtract, ALU.mult, [src, lst], [src])
                TT(src.ap, src.ap, lnp[:, gcol:gcol + 1024], ALU.mult, [src, lnp], [src])
                TT(dst_f32, src.ap, lnp[:, bcol:bcol + 1024], ALU.add, [src, lnp], [dst_buf])

            for grp in range(4):
                g0 = grp * 512
                gsl = slice(g0, g0 + 512)
                for tt in range(4):
                    i = grp * 4 + tt
                    xb = hx[tt]
                    DMA("sp", xb.ap, x[tok0 + i * 128: tok0 + (i + 1) * 128, :], [], [xb], xb)
                    for c in range(8):
                        pb = PB[c // 4]
                        TR(pb[:, (c % 4) * 128:(c % 4 + 1) * 128], xb[:, c * 128:(c + 1) * 128], identf, [xb, cfa], [pb])
                    CPA(xtgv[:, 0:4, tt * 128:(tt + 1) * 128], PB[0].ap.rearrange("p (c n) -> p c n", c=4), [PB[0]], [xtg])
                    CPV(xtgv[:, 4:8, tt * 128:(tt + 1) * 128], PB[1].ap.rearrange("p (c n) -> p c n", c=4), [PB[1]], [xtg])
                for cb2 in range(2):
                    bga, vga = wload(wga, 1024, cb2 * 512, 512)
                    buh, vuh = wload(wuh, 1024, cb2 * 512, 512)
                    bgb, vgb = wload(wgb, 1024, cb2 * 512, 512)
                    bun, vun = wload(wun, 1024, cb2 * 512, 512)
                    for cc in range(4):
                        csl = slice(cc * 128, (cc + 1) * 128)
                        oc_ = cb2 * 4 + cc
                        qa, qb, qc, qd_ = (PB[2], PB[3], PB[4], PB[5]) if cc % 2 == 0 else (PB[0], PB[1], PB[6], PB[7])
                        for c in range(8):
                            MM(qa.ap, vga[:, c, csl], xtgv[:, c, :], c == 0, c == 7, [bga, xtg], [qa])
                        for c in range(8):
                            MM(qb.ap, vuh[:, c, csl], aTv[:, c, gsl], c == 0, c == 7, [buh, bufB], [qb])
                        for c in range(8):
                            MM(qc.ap, vgb[:, c, csl], xtgv[:, c, :], c == 0, c == 7, [bgb, xtg], [qc])
                        for c in range(8):
                            MM(qd_.ap, vun[:, c, csl], bTv[:, c, gsl], c == 0, c == 7, [bun, bufA], [qd_])
                        ACT(sga.ap, qa.ap, AF.Sigmoid, [qa], [sga])
                        ACT(sgb.ap, qc.ap, AF.Sigmoid, [qc], [sgb])
                        TT(m1.ap, qb.ap, sga.ap, ALU.mult, [qb, sga], [m1])
                        TT(m2.ap, qd_.ap, sgb.ap, ALU.mult, [qd_, sgb], [m2])
                        TT(mgtv[:, oc_, :], m1.ap, m2.ap, ALU.add, [m1, m2], [mgt])
                bo0, vo0 = wload(wo, 1024, 0, 512)
                bo1, vo1 = wload(wo, 1024, 512, 512)
                for tt in range(4):
                    tl = slice(tt * 128, (tt + 1) * 128)
                    for hf, (bo, vo) in enumerate(((bo0, vo0), (bo1, vo1))):
                        pb = PB[2 * tt + hf]
                        for c in range(8):
                            MM(pb.ap, mgtv[:, c, tl], vo[:, c, :], c == 0, c == 7, [mgt, bo], [pb])
                for tt in range(4):
                    tl = slice(tt * 128, (tt + 1) * 128)
                    xb = hx[tt]
                    for hf in range(2):
                        pb = PB[2 * tt + hf]
                        STT(xb[:, hf * 512:(hf + 1) * 512], xb[:, hf * 512:(hf + 1) * 512], ALPHA, pb.ap,
                            ALU.mult, ALU.add, [xb, pb], [xb])
                    layer_norm(xb, 0, 1024, xb.ap, xb)
                    if dbg and s == 0:
                        i = grp * 4 + tt
                        DMA("sp", dbg_out["d_h1"][i * 128:(i + 1) * 128, :], xb.ap, [xb], [], xb, store=True)
                    CPA(hb.ap, xb.ap, [xb], [hb])
                    for c in range(8):
                        TR(PBb[c // 4][:, (c % 4) * 128:(c % 4 + 1) * 128], hb[:, c * 128:(c + 1) * 128], identb,
                           [hb, cb], [PB[c // 4]])
                    CPA(h1tv[:, 0:4, tl], PBb[0][:, 0:512].rearrange("p (c n) -> p c n", c=4), [PB[0]], [h1t])
                    CPV(h1tv[:, 4:8, tl], PBb[1][:, 0:512].rearrange("p (c n) -> p c n", c=4), [PB[1]], [h1t])
                for fb in range(6):
                    ncol = 512 if fb < 5 else 256
                    bg, vg = wload(wfg, DFF, fb * 512, ncol)
                    bu, vu = wload(wfu, DFF, fb * 512, ncol)
                    for cc in range(ncol // 128):
                        csl = slice(cc * 128, (cc + 1) * 128)
                        fc = fb * 4 + cc
                        pg, pu_ = PB[2 + 2 * (fc % 2)], PB[3 + 2 * (fc % 2)]
                        for c in range(8):
                            MM(pg.ap, vg[:, c, csl], h1tv[:, c, :], c == 0, c == 7, [bg, h1t], [pg])
                        for c in range(8):
                            MM(pu_.ap, vu[:, c, csl], h1tv[:, c, :], c == 0, c == 7, [bu, h1t], [pu_])
                        ACT(sga.ap, pg.ap, AF.Silu, [pg], [sga])
                        TT(acttv[:, fc, :], sga.ap, pu_.ap, ALU.mult, [sga, pu_], [actt])
                for db in range(6):
                    nk = 4 if db < 5 else 2
                    b = ring[rc[0] % 4]
                    rc[0] += 1
                    v = b.ap[:, 0:nk * 1024].rearrange("p (k n) -> p k n", k=nk)
                    DMA("pool", v, wfd[:, db * 4:db * 4 + nk, :], [], [b], b)
                    for tt in range(4):
                        tl = slice(tt * 128, (tt + 1) * 128)
                        for kq_ in range(nk):
                            kidx = db * 4 + kq_
                            for hf in range(2):
                                pb = PB[tt * 2 + hf]
                                MM(pb.ap, acttv[:, kidx, tl], v[:, kq_, hf * 512:(hf + 1) * 512], kidx == 0, kidx == 21,
                                   [actt, b], [pb])
                for tt in range(4):
                    xb = hx[tt]
                    for hf in range(2):
                        pb = PB[tt * 2 + hf]
                        STT(xb[:, hf * 512:(hf + 1) * 512], xb[:, hf * 512:(hf + 1) * 512], ALPHA, pb.ap,
                            ALU.mult, ALU.add, [xb, pb], [xb])
                for tt in range(4):
                    i = grp * 4 + tt
                    xb = hx[tt]
                    layer_norm(xb, 2048, 3072, xb.ap, xb)
                    DMA("sp", y[tok0 + i * 128: tok0 + (i + 1) * 128, :], xb.ap, [xb], [], xb, store=True)

        S.finish()
        S.emit()
    return nc


def _pc(M):
    n = M.shape[1]
    return np.ascontiguousarray(M.reshape(M.shape[0] // 128, 128, n).transpose(1, 0, 2))


def prep_common(inp):
    f32 = np.float32
    W = np.asarray(inp["w_in"], f32)[0]
    o = np.cumsum([0, 1024, 1024, 1024, 1024, 1024, 256, 256, 256, 256, 256, 256, 48, 1024, 1024])
    hq, hf, hi, hg, nq, kc, vc, ks, vs, kw, vw, ng, ga, gb = [int(v) for v in o[:14]]
    d = {}
    d["whg"] = np.stack([_pc(np.concatenate([W[:, hq + h * 128: hq + (h + 1) * 128], W[:, hf + h * 128: hf + (h + 1) * 128],
                                             W[:, hi + h * 128: hi + (h + 1) * 128], W[:, hg + h * 128: hg + (h + 1) * 128]], 1))
                         for h in range(8)])
    cols = []
    for k in range(4):
        sl = lambda b: W[:, b + k * 64: b + (k + 1) * 64]
        cols += [sl(ks), sl(kw), sl(kc), sl(vc), sl(vs), sl(vw)]
    d["wkv"] = _pc(np.concatenate(cols, 1))
    d["wq"] = _pc(W[:, nq:nq + 1024])
    d["wng"] = _pc(W[:, ng:ng + 48])
    d["wga"] = _pc(W[:, ga:ga + 1024])
    d["wgb"] = _pc(W[:, gb:gb + 1024])
    d["wuh"] = _pc(np.asarray(inp["w_up_hg"], f32)[0])
    d["wun"] = _pc(np.asarray(inp["w_up_nsa"], f32)[0])
    d["wo"] = _pc(np.asarray(inp["w_o"], f32)[0])
    d["wfg"] = _pc(np.asarray(inp["w_ffn_gate"], f32)[0])
    d["wfu"] = _pc(np.asarray(inp["w_ffn_up"], f32)[0])
    d["wfd"] = _pc(np.asarray(inp["w_ffn_down"], f32)[0])
    w1k = np.asarray(inp["cmp_k_w1"], f32)[0].reshape(32, 64, 256).transpose(1, 0, 2)
    w1v = np.asarray(inp["cmp_v_w1"], f32)[0].reshape(32, 64, 256).transpose(1, 0, 2)
    d["w1"] = np.ascontiguousarray(np.concatenate([w1k, w1v], 0))
    d["w2k"] = np.ascontiguousarray(np.asarray(inp["cmp_k_w2"], f32)[0].reshape(2, 128, 64).transpose(1, 0, 2))
    d["w2v"] = np.ascontiguousarray(np.asarray(inp["cmp_v_w2"], f32)[0].reshape(2, 128, 64).transpose(1, 0, 2))
    cfa = np.zeros((128, NCFA), f32)
    cfa[:, C_ID:C_ID + 128] = np.eye(128, dtype=f32)
    sidx = np.arange(128)[:, None]
    tidx = np.arange(128)[None, :]
    same = (sidx // 64) == (tidx // 64)
    cfa[:, C_TRI:C_TRI + 128] = (same & (sidx <= tidx)).astype(f32)
    cfa[:, C_TREV:C_TREV + 128] = (same & (sidx > tidx)).astype(f32)
    cfa[:, C_CI] = (np.arange(128) < 64)
    cfa[:, C_CI + 1] = (np.arange(128) >= 64)
    p = np.arange(128)[:, None]
    j = np.arange(8)[None, :]
    cfa[:, C_M8:C_M8 + 8] = np.where(p >= 16 * j + 15, 0.0, -1e30)
    cfa[:, C_INVF:C_INVF + 32] = (10000.0 ** (-np.arange(32, dtype=f32) / 32)).astype(f32)[None, :]
    cfa[:, C_NG:C_NG + 128] = np.asarray(inp["hg_norm_g"], f32)[0][None, :]
    b1k = np.asarray(inp["cmp_k_b1"], f32)[0]
    b1v = np.asarray(inp["cmp_v_b1"], f32)[0]
    cfa[:, C_B1 + 0] = b1k[0:128]
    cfa[:, C_B1 + 1] = b1k[128:256]
    cfa[:, C_B1 + 2] = b1v[0:128]
    cfa[:, C_B1 + 3] = b1v[128:256]
    cfa[0:64, C_B2K] = np.asarray(inp["cmp_k_b2"], f32)[0]
    v01 = np.zeros((128, 16, 32), f32)
    fb2 = np.zeros((128, 16, 32), f32)
    blk = np.arange(32)[None, :]
    for i in range(16):
        cur = (2 * i + (np.arange(128) >= 64))[:, None]
        valid = blk <= cur
        forced = (blk == 0) | (blk == cur) | (blk == cur - 1)
        v01[:, i, :] = valid
        fb2[:, i, :] = np.where(valid, 1000.0 * forced, -1.0)
    cfa[:, C_V01:C_V01 + 512] = v01.reshape(128, 512)
    cfa[:, C_FB2:C_FB2 + 512] = fb2.reshape(128, 512)
    cfa[:, C_E6] = 1e-6
    cfa[:, C_E5] = 1e-5
    cfa[:, C_ONE] = 1.0
    d["cfa"] = cfa
    cb = np.zeros((128, NCB), f32)
    cb[:, B_ID:B_ID + 128] = np.eye(128, dtype=f32)
    cs = np.arange(127)[:, None] * 16
    bs = np.arange(32)[None, :] * 64
    ov = np.clip(np.minimum(cs + 32, bs + 64) - np.maximum(cs, bs), 0, None) / 32.0
    cb[0:127, B_OV:B_OV + 32] = ov
    E = np.zeros((32, 16, 128), f32)
    for jj in range(16):
        for key in range(128):
            E[2 * jj + key // 64, jj, key] = 1.0
    cb[0:32, B_E:B_E + 2048] = E.reshape(32, 2048)
    kq = np.arange(128)
    caus = np.where(kq[:, None] > kq[None, :], -30000.0, 0.0).astype(f32)
    anti = np.where(kq[:, None] <= kq[None, :], -30000.0, 0.0).astype(f32)
    cb[:, B_CAUS:B_CAUS + 512] = np.tile(caus, (1, 4))
    cb[:, B_ANTI:B_ANTI + 512] = np.tile(anti, (1, 4))
    cb[:, B_ONES:B_ONES + 128] = 1.0
    cb[0:64, B_POST:B_POST + 32] = np.asarray(inp["cmp_k_pos"], f32)[0].T
    cb[64:128, B_POST:B_POST + 32] = np.asarray(inp["cmp_v_pos"], f32)[0].T
    cb[:, B_B2V:B_B2V + 64] = np.asarray(inp["cmp_v_b2"], f32)[0][None, :]
    cb[:, B_TRI:B_TRI + 128] = cfa[:, C_TRI:C_TRI + 128]
    cb[:, B_TREV:B_TREV + 128] = cfa[:, C_TREV:C_TREV + 128]
    cb[:, B_CI:B_CI + 2] = cfa[:, C_CI:C_CI + 2]
    d["cb"] = cb
    lb = np.asarray(inp["hg_lb_logits"], f32)
    d["lbrows"] = np.ascontiguousarray(np.broadcast_to(lb.reshape(1, 2048), (128, 2048)))
    lnp = np.concatenate([np.asarray(inp[k], f32)[0] for k in ("ln1_g", "ln1_b", "ln2_g", "ln2_b")])
    d["lnp"] = np.ascontiguousarray(np.broadcast_to(lnp[None, :], (128, 4096)))
    return d


_NC_CACHE = {}


def kernel(**inp):
    x = np.asarray(inp["x"], np.float32)
    pos = np.asarray(inp["positions"], np.int32)
    common = prep_common(inp)
    nseq = x.shape[0] // NCORES
    if nseq not in _NC_CACHE:
        _NC_CACHE[nseq] = build(nseq)
    nc = _NC_CACHE[nseq]
    in_maps = []
    for c in range(NCORES):
        m = dict(common)
        m["x"] = np.ascontiguousarray(x[c * nseq:(c + 1) * nseq].reshape(nseq * SEQ, DM))
        m["pos"] = np.ascontiguousarray(pos[c * nseq:(c + 1) * nseq].reshape(nseq, NT, 128).transpose(0, 2, 1))
        in_maps.append(m)
    res = run_bass_kernel_spmd(nc, in_maps, core_ids=list(range(NCORES)))
    out = np.concatenate([r["y"].reshape(nseq, SEQ, DM) for r in res.results], axis=0)
    return out.astype(np.float32)
```

```python
import numpy as np
import concourse.bass as bass
import concourse.mybir as mybir
from concourse.bass_utils import run_bass_kernel_spmd
from contextlib import ExitStack

F32 = mybir.dt.float32
BF16 = mybir.dt.bfloat16
I32 = mybir.dt.int32
AF = mybir.ActivationFunctionType
ALU = mybir.AluOpType

NCORES = 8
SEQ = 2048
NT = 16
DM = 1024
DFF = 2816
ALPHA = 2.0 ** 0.25
TWO_PI = float(2 * np.pi)


class Reg:
    _n = 0

    def __init__(self, name=None):
        Reg._n += 1
        self.name = (name or "r") + f"_{Reg._n}"
        self.writers = {}
        self.readers = {}
        self.dma_sem = None
        self.dma_total = 0


class Sched:
    ENG = ["pe", "act", "dve", "pool", "sp"]

    def __init__(self, nc, es):
        self.nc = nc
        self.es = es
        self.sem = {e: es.enter_context(nc.semaphore("s_" + e)) for e in self.ENG}
        self.cnt = {e: 0 for e in self.ENG}
        self.waited = {e: {} for e in self.ENG}
        self.stream = {e: [] for e in self.ENG}
        self.stores = {}
        self.dmakeys = {}

    def dma_sem(self, R):
        if R.dma_sem is None:
            R.dma_sem = self.es.enter_context(self.nc.semaphore("d_" + R.name))
        return R.dma_sem

    def _need(self, e, key, sv, waits):
        if e == "pe" and key == "pe":
            return
        if self.waited[e].get(key, 0) >= sv[1]:
            return
        if key not in waits or waits[key][1] < sv[1]:
            waits[key] = sv

    def op(self, e, fn, reads=(), writes=(), dma=None, store=False):
        waits = {}
        for r in reads:
            for k, sv in r.writers.items():
                self._need(e, k, sv, waits)
        for w in writes:
            for k, sv in w.writers.items():
                self._need(e, k, sv, waits)
            for k, sv in w.readers.items():
                self._need(e, k, sv, waits)
        for k, sv in waits.items():
            self.waited[e][k] = sv[1]
        if dma is None:
            self.cnt[e] += 1
            key = e
            sv = (self.sem[e], self.cnt[e])
            inc = (self.sem[e], 1)
        else:
            s = self.dma_sem(dma)
            dma.dma_total += 16
            key = "d_" + dma.name
            sv = (s, dma.dma_total)
            inc = (s, 16)
            self.dmakeys[key] = sv
            if store:
                self.stores[key] = sv
        for r in reads:
            if key not in r.readers or r.readers[key][1] < sv[1]:
                r.readers[key] = sv
        for w in writes:
            w.writers = {key: sv}
            w.readers = {}
        self.stream[e].append((list(waits.values()), fn, inc))

    def barrier(self):
        for e in self.ENG:
            waits = []
            for f in self.ENG:
                if f != e and self.cnt[f] > self.waited[e].get(f, 0):
                    waits.append((self.sem[f], self.cnt[f]))
                    self.waited[e][f] = self.cnt[f]
            for k, sv in self.dmakeys.items():
                if self.waited[e].get(k, 0) < sv[1]:
                    waits.append(sv)
                    self.waited[e][k] = sv[1]
            self.stream[e].append((waits, None, None))

    def finish(self):
        waits = []
        for k, sv in self.stores.items():
            if self.waited["sp"].get(k, 0) < sv[1]:
                waits.append(sv)
        self.stream["sp"].append((waits, None, None))

    def emit(self):
        nc = self.nc
        with nc.Block() as block:
            def mk(e):
                def body(eng):
                    for waits, fn, inc in self.stream[e]:
                        for s, v in waits:
                            eng.wait_ge(s, v)
                        if fn is not None:
                            fn(eng).then_inc(inc[0], inc[1])
                return body
            block.tensor(mk("pe"))
            block.scalar(mk("act"))
            block.vector(mk("dve"))
            block.gpsimd(mk("pool"))
            block.sync(mk("sp"))


class Buf:
    def __init__(self, ap, name):
        self.ap = ap
        self.r = Reg(name)

    def __getitem__(self, k):
        return self.ap[k]


def bmid(a, k):
    return bass.AP(a.tensor, a.offset, [list(a.ap[0]), [0, k], list(a.ap[1])])


def blast(a, k):
    return bass.AP(a.tensor, a.offset, [list(a.ap[0]), list(a.ap[1]), [0, k]])


C_ID, C_TRI, C_TREV, C_CI, C_M8, C_INVF, C_NG, C_B1, C_B2K, C_V01, C_FB2, C_E6, C_E5, C_ONE, NCFA = \
    0, 128, 256, 384, 386, 394, 426, 554, 558, 560, 1072, 1584, 1585, 1586, 1592
B_ID, B_OV, B_E, B_CAUS, B_ANTI, B_ONES, B_POST, B_B2V, B_TRI, B_TREV, B_CI, NCB = \
    0, 128, 160, 2208, 2720, 3232, 3360, 3392, 3456, 3584, 3712, 3720

ARENA_BYTES = 207 * 1024
O_CFA = 0
O_CB = O_CFA + NCFA * 4
O_ROML = O_CB + NCB * 2
O_A = ((O_ROML + 4096 + 1023) // 1024) * 1024
O_B = O_A + 32768
O_XT = O_B + 32768
O_KSW = O_XT + 32768
O_VSW = O_KSW + 16384
O_KCMP = O_VSW + NT * 4 * 2 * 65 * 2
O_VCMP = O_KCMP + 1024
O_C2 = O_VCMP + 512
O_S2 = O_C2 + 4096
O_SCR = O_S2 + 4096


def build(NSEQ=4, dbg=False, stop=9, skip=(), npair=4, ntile=NT, hgcut=99):
    nc = bass.Bass("TRN2", target_bir_lowering=False)

    def din(name, shape, dt=F32):
        return nc.dram_tensor(name, shape, dt, kind="ExternalInput").ap()

    x = din("x", [NSEQ * SEQ, DM])
    pos = din("pos", [NSEQ, 128, NT], I32)
    wkv = din("wkv", [128, 8, 1536])
    wq = din("wq", [128, 8, 1024])
    wng = din("wng", [128, 8, 48])
    whg = din("whg", [8, 128, 8, 512])
    wga = din("wga", [128, 8, 1024])
    wgb = din("wgb", [128, 8, 1024])
    wuh = din("wuh", [128, 8, 1024])
    wun = din("wun", [128, 8, 1024])
    wo = din("wo", [128, 8, 1024])
    wfg = din("wfg", [128, 8, DFF])
    wfu = din("wfu", [128, 8, DFF])
    wfd = din("wfd", [128, 22, 1024])
    w1 = din("w1", [128, 32, 256])
    w2k = din("w2k", [128, 2, 64])
    w2v = din("w2v", [128, 2, 64])
    cfa_d = din("cfa", [128, NCFA])
    cb_d = din("cb", [128, NCB])
    lbrows = din("lbrows", [128, 2048])
    lnp_d = din("lnp", [128, 4096])
    y = nc.dram_tensor("y", [NSEQ * SEQ, DM], F32, kind="ExternalOutput").ap()
    dbg_out = {}
    if dbg:
        for nm in ("d_aT", "d_bT"):
            dbg_out[nm] = nc.dram_tensor(nm, [128, 8 * SEQ], F32, kind="ExternalOutput").ap()
        dbg_out["d_h1"] = nc.dram_tensor("d_h1", [SEQ, DM], F32, kind="ExternalOutput").ap()

    es = ExitStack()
    with es:
        S = Sched(nc, es)
        arena = es.enter_context(nc.sbuf_tensor("arena", [128, ARENA_BYTES // 2], BF16))
        arena32 = arena.bitcast(F32)
        arenaI = arena.bitcast(I32)
        psf = [es.enter_context(nc.psum_tensor(f"ps{i}", [128, 512], F32)) for i in range(8)]
        PB = [Buf(p[:, :], f"ps{i}") for i, p in enumerate(psf)]
        PBb = [p.bitcast(BF16)[:, :] for p in psf]

        _bufs = {}

        def at(off, n, dt, name):
            key = (off, n, str(dt), name)
            if key not in _bufs:
                _bufs[key] = _at(off, n, dt, name)
            return _bufs[key]

        def _at(off, n, dt, name):
            assert off % 4 == 0 and off + n * (2 if dt == BF16 else 4) <= ARENA_BYTES, (name, off, n)
            if dt == BF16:
                return Buf(arena[:, off // 2: off // 2 + n], name)
            return Buf((arenaI if dt == I32 else arena32)[:, off // 4: off // 4 + n], name)

        class Cur:
            def __init__(self, off, lim=ARENA_BYTES):
                self.off = off
                self.lim = lim

            def take(self, n, dt, name):
                nb = n * (2 if dt == BF16 else 4)
                b = at(self.off, n, dt, name)
                self.off += (nb + 63) // 64 * 64
                assert self.off <= self.lim, (name, self.off, self.lim)
                return b

        def regs(bs):
            return [b.r if isinstance(b, Buf) else b for b in bs]

        def MM(out, lhsT, rhs, start, stop, R, W, skip=False):
            S.op("pe", lambda e: e.matmul(out, lhsT, rhs, start=start, stop=stop, skip_group_check=skip),
                 regs(R), regs(W))

        def TR(out, in_, ident, R, W):
            S.op("pe", lambda e: e.transpose(out, in_, ident), regs(R), regs(W))

        def ACT(out, in_, func, R, W, bias=None, scale=None, accum=None):
            kw = {}
            if bias is not None:
                kw["bias"] = bias
            if scale is not None:
                kw["scale"] = scale
            if accum is not None:
                kw["accum_out"] = accum
            S.op("act", lambda e: e.activation(out=out, in_=in_, func=func, **kw), regs(R), regs(W))

        def TT(out, in0, in1, op, R, W, eng="dve"):
            S.op(eng, lambda e: e.tensor_tensor(out=out, in0=in0, in1=in1, op=op), regs(R), regs(W))

        def TS(out, in0, s1, s2, op0, op1, R, W, eng="dve"):
            if s2 is None:
                S.op(eng, lambda e: e.tensor_scalar(out=out, in0=in0, scalar1=s1, scalar2=None, op0=op0),
                     regs(R), regs(W))
            else:
                S.op(eng, lambda e: e.tensor_scalar(out=out, in0=in0, scalar1=s1, scalar2=s2, op0=op0, op1=op1),
                     regs(R), regs(W))

        def STT(out, in0, scalar, in1, op0, op1, R, W):
            S.op("dve", lambda e: e.scalar_tensor_tensor(out=out, in0=in0, scalar=scalar, in1=in1, op0=op0, op1=op1),
                 regs(R), regs(W))

        def CPV(out, in_, R, W):
            S.op("dve", lambda e: e.tensor_copy(out, in_), regs(R), regs(W))

        def CPA(out, in_, R, W):
            ACT(out, in_, AF.Copy, R, W)

        def RCP(out, in_, R, W):
            S.op("dve", lambda e: e.reciprocal(out, in_), regs(R), regs(W))

        def MSET(out, val, W, eng="dve"):
            S.op(eng, lambda e: e.memset(out, val), [], regs(W))

        def DMA(q, out, in_, R, W, semb, store=False):
            S.op(q, lambda e: e.dma_start(out=out, in_=in_), regs(R), regs(W), dma=semb.r, store=store)

        cfa = at(O_CFA, NCFA, F32, "cfa")
        cb = at(O_CB, NCB, BF16, "cb")
        roml = at(O_ROML, 1024, F32, "roml")
        bufA = at(O_A, 8 * SEQ, BF16, "A")
        bufB = at(O_B, 8 * SEQ, BF16, "B")
        xT = at(O_XT, 8 * SEQ, BF16, "xT")
        ksw = at(O_KSW, 4 * SEQ, BF16, "ksw")
        vsw = at(O_VSW, NT * 4 * 2 * 65, BF16, "vsw")
        kcmp = at(O_KCMP, 4 * 128, BF16, "kcmp")
        vcmp = at(O_VCMP, 4 * 64, BF16, "vcmp")
        c2 = at(O_C2, NT * 64, F32, "c2")
        s2 = at(O_S2, NT * 64, F32, "s2")
        aTv = bufB.ap.rearrange("p (c n) -> p c n", c=8)
        bTv = bufA.ap.rearrange("p (c n) -> p c n", c=8)
        xTv = xT.ap.rearrange("p (c n) -> p c n", c=8)
        kswv = ksw.ap.rearrange("p (h n) -> p h n", h=4)
        vswv = vsw.ap.rearrange("p (t h b d) -> p t h b d", t=NT, h=4, b=2)
        kcmpv = kcmp.ap.rearrange("p (h n) -> p h n", h=4)
        vcmpv = vcmp.ap.rearrange("p (h d) -> p h d", h=4)
        c2v = c2.ap.rearrange("p (t d) -> p t d", t=NT)
        s2v = s2.ap.rearrange("p (t d) -> p t d", t=NT)
        identf = cfa[:, C_ID:C_ID + 128]
        identb = cb[:, B_ID:B_ID + 128]

        DMA("sp", cfa.ap, cfa_d, [], [cfa], cfa)
        DMA("pool", cb.ap, cb_d, [], [cb], cb)
        ci = Cur(O_SCR)
        lbt = ci.take(2048, F32, "lbt")
        DMA("sp", lbt.ap, lbrows, [], [lbt], lbt)
        TT(lbt[:, 0:1024], lbt[:, 0:1024], lbt[:, 1024:2048], ALU.subtract, [lbt], [lbt])
        ACT(roml.ap, lbt[:, 0:1024], AF.Exp, [lbt], [roml])
        TS(roml.ap, roml.ap, 1.0, None, ALU.add, None, [roml], [roml])

        def rope(src_ps, srcbuf, nh, tile, t1, t2, dst_list):
            sv = src_ps.rearrange("p (h t d) -> p h t d", h=nh, t=2)
            t1v = t1.ap[:, 0:nh * 64].rearrange("p (h d) -> p h d", h=nh)
            t2v = t2.ap[:, 0:nh * 64].rearrange("p (h t d) -> p h t d", h=nh, t=2)
            TT(t1v, src_ps.rearrange("p (h d) -> p h d", h=nh), bmid(c2v[:, tile, :], nh), ALU.mult,
               [srcbuf, c2], [t1])
            TT(t2v[:, :, 0, :], sv[:, :, 1, :], bmid(s2v[:, tile, 0:32], nh), ALU.mult, [srcbuf, s2], [t2])
            TT(t2v[:, :, 1, :], sv[:, :, 0, :], bmid(s2v[:, tile, 32:64], nh), ALU.mult, [srcbuf, s2, t2], [t2])
            t2f = t2.ap[:, 0:nh * 64].rearrange("p (h d) -> p h d", h=nh)
            for dap, dbuf in dst_list:
                TT(dap, t1v, t2f, ALU.add, [t1, t2], [dbuf])

        for s in range(NSEQ):
            tok0 = s * SEQ
            S.barrier()
            ct = at(O_A, 4 * SEQ, BF16, "ct")
            ctv = ct.ap.rearrange("p (h n) -> p h n", h=4)
            w1s = at(O_A + 16384, 32 * 256, BF16, "w1s")
            w1v_ = w1s.ap.rearrange("p (l n) -> p l n", l=32)
            wkvs = at(O_B, 8 * 1536, BF16, "wkvs")
            wkvv = wkvs.ap.rearrange("p (c n) -> p c n", c=8)
            MSET(vsw.ap, 1.0, [vsw])
            DMA("pool", wkvv, wkv, [], [wkvs], wkvs)
            DMA("pool", w1v_, w1, [], [w1s], w1s)
            cu = Cur(O_SCR)
            w2ks = cu.take(128, BF16, "w2ks")
            w2vs = cu.take(128, BF16, "w2vs")
            DMA("pool", w2ks.ap.rearrange("p (h d) -> p h d", h=2), w2k, [], [w2ks], w2ks)
            DMA("pool", w2vs.ap.rearrange("p (h d) -> p h d", h=2), w2v, [], [w2vs], w2vs)
            posi = cu.take(NT, I32, "posi")
            posf = cu.take(NT, F32, "posf")
            ang = cu.take(NT * 32, F32, "ang")
            ang2 = cu.take(NT * 32, F32, "ang2")
            kq = cu.take(NT * 32, F32, "kq")
            kqi = cu.take(NT * 32, I32, "kqi")
            DMA("sp", posi.ap, pos[s], [], [posi], posi)
            CPV(posf.ap, posi.ap, [posi], [posf])
            angv = ang.ap.rearrange("p (t j) -> p t j", t=NT)
            TT(angv, blast(posf.ap, 32), bmid(cfa[:, C_INVF:C_INVF + 32], NT), ALU.mult, [posf, cfa], [ang])
            TS(ang2.ap, ang.ap, float(np.pi / 2), None, ALU.add, None, [ang], [ang2])
            for src, which in ((ang, 0), (ang2, 1)):
                TS(kq.ap, src.ap, 1.0 / TWO_PI, None, ALU.mult, None, [src], [kq])
                CPV(kqi.ap, kq.ap, [kq], [kqi])
                CPV(kq.ap, kqi.ap, [kqi], [kq])
                STT(src.ap, kq.ap, -TWO_PI, src.ap, ALU.mult, ALU.add, [kq, src], [src])
                TS(src.ap, src.ap, 3.1415925, -3.1415925, ALU.min, ALU.max, [src], [src])
                srcv = src.ap.rearrange("p (t j) -> p t j", t=NT)
                if which == 0:
                    ACT(s2v[:, :, 32:64], srcv, AF.Sin, [src], [s2])
                    TS(s2v[:, :, 0:32], s2v[:, :, 32:64], -1.0, None, ALU.mult, None, [s2], [s2])
                else:
                    ACT(c2v[:, :, 0:32], srcv, AF.Sin, [src], [c2])
                    CPV(c2v[:, :, 32:64], c2v[:, :, 0:32], [c2], [c2])
            xin = [cu.take(1024, F32, f"xin{i}") for i in range(2)]
            t1 = cu.take(256, F32, "t1")
            t2 = cu.take(256, F32, "t2")
            krc = cu.take(256, BF16, "krc")
            krcv = krc.ap.rearrange("p (h d) -> p h d", h=4)
            for i in range(ntile if 1 in skip else NT):
                xb = xin[i % 2]
                DMA("sp", xb.ap, x[tok0 + i * 128: tok0 + (i + 1) * 128, :], [], [xb], xb)
                for c in range(8):
                    pb = PB[c // 4]
                    TR(pb[:, (c % 4) * 128:(c % 4 + 1) * 128], xb[:, c * 128:(c + 1) * 128], identf, [xb, cfa], [pb])
                CPA(xTv[:, 0:4, i * 128:(i + 1) * 128], PB[0].ap.rearrange("p (c n) -> p c n", c=4), [PB[0]], [xT])
                CPV(xTv[:, 4:8, i * 128:(i + 1) * 128], PB[1].ap.rearrange("p (c n) -> p c n", c=4), [PB[1]], [xT])
                for hk in range(4):
                    pb = PB[2 + hk]
                    for c in range(8):
                        MM(pb[:, 0:384], xTv[:, c, i * 128:(i + 1) * 128], wkvv[:, c, hk * 384:(hk + 1) * 384],
                           c == 0, c == 7, [xT, wkvs], [pb])
                for hk in range(4):
                    pb = PB[2 + hk]
                    rope(pb[:, 0:192], pb, 3, i, t1, t2, [(krcv[:, 0:3, :], krc)])
                    CPA(krc[:, 192:256], pb[:, 192:256], [pb], [krc])
                    CPA(vswv[:, i, hk, :, 0:64], pb[:, 256:384].rearrange("p (b d) -> p b d", b=2), [pb], [vsw])
                    TR(PBb[7][:, 0:128], krc[:, 0:128], identb, [krc, cb], [PB[7]])
                    TR(PBb[7][:, 128:256], krc[:, 128:256], identb, [krc, cb], [PB[7]])
                    CPV(kswv[:, hk, i * 128:(i + 1) * 128], PBb[7][:, 0:128], [PB[7]], [ksw])
                    CPA(ctv[:, hk, i * 128:(i + 1) * 128], PBb[7][:, 128:256], [PB[7]], [ct])
            c1 = cu.take(4, F32, "c1")
            hdn = cu.take(4 * 128, BF16, "hdn")
            hdnv = hdn.ap.rearrange("p (a n) -> p a n", a=4)
            for kv in range(2):
                pb = PB[kv]
                pr = slice(64 * kv, 64 * kv + 64)
                for half in range(2):
                    col = kv * 2 + half
                    for l in range(32):
                        MM(pb[:, col:col + 1], w1v_[pr, l, half * 128:(half + 1) * 128],
                           cb[pr, B_POST + l:B_POST + l + 1], l == 0, l == 31, [w1s, cb], [pb])
                TT(c1[:, 2 * kv:2 * kv + 2], pb[:, 2 * kv:2 * kv + 2], cfa[:, C_B1 + 2 * kv:C_B1 + 2 * kv + 2], ALU.add,
                   [pb, cfa], [c1])
            for hk in range(0 if 1 in skip else 4):
                for kv in range(2):
                    pr = slice(64 * kv, 64 * kv + 64)
                    for half in range(2):
                        col = kv * 2 + half
                        pb = PB[1 + (col % 2)]
                        for l in range(32):
                            MM(pb[:, 0:127], w1v_[pr, l, half * 128:(half + 1) * 128],
                               ctv[pr, hk, l:l + 2017:16], l == 0, l == 31, [w1s, ct], [pb])
                        ACT(hdnv[:, col, 0:127], pb[:, 0:127], AF.Silu, [pb, c1], [hdn], bias=c1[:, col:col + 1])
                pb = PB[3]
                w2kv = w2ks.ap.rearrange("p (h d) -> p h d", h=2)
                w2vv = w2vs.ap.rearrange("p (h d) -> p h d", h=2)
                for half in range(2):
                    MM(pb[0:64, 0:127], w2kv[:, half, :], hdnv[:, half, 0:127], half == 0, half == 1, [w2ks, hdn], [pb])
                ACT(kcmpv[0:64, hk, 0:127], pb[0:64, 0:127], AF.Identity, [pb, cfa], [kcmp],
                    bias=cfa[0:64, C_B2K:C_B2K + 1])
                pb = PB[4]
                for half in range(2):
                    MM(pb[0:127, 0:64], hdnv[:, 2 + half, 0:127], w2vv[:, half, :], half == 0, False, [w2vs, hdn], [pb])
                MM(pb[0:127, 0:64], cb[0:1, B_ONES:B_ONES + 127], cb[0:1, B_B2V:B_B2V + 64], False, True, [cb], [pb])
                CPV(vcmpv[0:127, hk, :], pb[0:127, 0:64], [pb], [vcmp])

            if stop < 2:
                continue
            S.barrier()
            cu = Cur(O_SCR)
            wqs = cu.take(8 * 1024, BF16, "wqs")
            wqv = wqs.ap.rearrange("p (c n) -> p c n", c=8)
            wngs = cu.take(8 * 48, BF16, "wngs")
            wngv = wngs.ap.rearrange("p (c n) -> p c n", c=8)
            DMA("pool", wqv, wq, [], [wqs], wqs)
            DMA("pool", wngv, wng, [], [wngs], wngs)
            t1 = cu.take(256, F32, "t1a")
            t2 = cu.take(256, F32, "t2a")
            qr2 = cu.take(512, BF16, "qr2")
            qr2v = qr2.ap.rearrange("p (g b d) -> p g b d", g=4, b=2)
            qt2 = cu.take(512, BF16, "qt2")
            qt2v = qt2.ap.rearrange("p (g n) -> p g n", g=4)
            ec = cu.take(512, F32, "ec")
            ecv = ec.ap.rearrange("p (g n) -> p g n", g=4)
            pcb = cu.take(512, BF16, "pcb")
            pcbv = pcb.ap.rearrange("p (g n) -> p g n", g=4)
            pct = cu.take(512, BF16, "pct")
            pctv = pct.ap.rearrange("p (g n) -> p g n", g=4)
            st = cu.take(64, F32, "st")
            sc = cu.take(32, F32, "sc")
            nsl = cu.take(32, BF16, "nsl")
            rb = cu.take(512, BF16, "rb")
            rbv = rb.ap.rearrange("p (g n) -> p g n", g=4)
            pt = [cu.take(512, BF16, f"pt{i}") for i in range(2)]
            sg = cu.take(48, F32, "sg")
            oc = cu.take(256, F32, "oc")
            fac = cu.take(8, F32, "fac")
            btile = cu.take(256, BF16, "btile")
            acc = cu.take(256, F32, "acc")
            tmp = cu.take(256, F32, "tmpc")
            Ev = cb[0:32, B_E:B_E + 2048].rearrange("p (j n) -> p j n", j=16)
            qt2s = [qt2, cu.take(512, BF16, "qt2b")]
            rbs = [rb, cu.take(512, BF16, "rbb")]
            ocs = [oc, cu.take(256, F32, "ocb")]
            sgs = [sg, cu.take(48, F32, "sgb")]
            iters = [(i, hk) for i in range(0 if 2 in skip else NT) for hk in range(4)]

            def att_front(n):
                i, hk = iters[n]
                p = n % 2
                tsl = slice(i * 128, (i + 1) * 128)
                ncv = min(127, 8 * i + 7)
                qt2_, rb_, oc_, sg_ = qt2s[p], rbs[p], ocs[p], sgs[i % 2]
                qt2v_ = qt2_.ap.rearrange("p (g n) -> p g n", g=4)
                rbv_ = rb_.ap.rearrange("p (g n) -> p g n", g=4)
                if hk == 0:
                    pb = PB[0]
                    for c in range(8):
                        MM(pb[:, 0:48], xTv[:, c, tsl], wngv[:, c, :], c == 0, c == 7, [xT, wngs], [pb])
                    ACT(sg_.ap, pb[:, 0:48], AF.Exp, [pb], [sg_], scale=-1.0)
                    TS(sg_.ap, sg_.ap, 1.0, None, ALU.add, None, [sg_], [sg_])
                    RCP(sg_.ap, sg_.ap, [sg_], [sg_])
                pb = PB[1]
                for c in range(8):
                    MM(pb[:, 0:256], xTv[:, c, tsl], wqv[:, c, hk * 256:(hk + 1) * 256], c == 0, c == 7,
                       [xT, wqs], [pb])
                rope(pb[:, 0:256], pb, 4, i, t1, t2, [(qr2v[:, :, 0, :], qr2), (qr2v[:, :, 1, :], qr2)])
                yield
                for g in range(4):
                    TR(PBb[7][:, g * 128:(g + 1) * 128], qr2[:, g * 128:(g + 1) * 128], identb, [qr2, cb], [PB[7]])
                CPA(qt2_.ap, PBb[7][:, 0:512], [PB[7]], [qt2_])
                yield
                ps = PB[0]
                psv = ps.ap.rearrange("p (g n) -> p g n", g=4)
                for g in range(4):
                    MM(psv[:, g, 0:ncv], qt2v_[0:64, g, :], kcmpv[0:64, hk, 0:ncv], True, True, [qt2_, kcmp], [ps])
                j0 = 1 if i == 0 else 0
                lo = ncv - (8 - j0)
                TT(psv[:, :, lo:ncv], psv[:, :, lo:ncv], bmid(cfa[:, C_M8 + j0:C_M8 + 8], 4), ALU.add,
                   [ps, cfa], [ps])
                S.op("dve", lambda e, o=st[:, 0:4], a=psv[:, :, 0:ncv]: e.tensor_reduce(
                    out=o, in_=a, axis=mybir.AxisListType.X, op=ALU.max), [ps.r], [st.r])
                TS(st[:, 4:8], st[:, 0:4], -1e4, -0.125, ALU.max, ALU.mult, [st], [st])
                MSET(st[:, 8:12], 0.0, [st])
                for g in range(4):
                    ACT(ecv[:, g, 0:ncv], psv[:, g, 0:ncv], AF.Exp, [ps, st], [ec, st],
                        bias=st[:, 4 + g:5 + g], scale=0.125, accum=st[:, 8 + g:9 + g])
                TS(st[:, 12:16], st[:, 8:12], 1e-30, None, ALU.max, None, [st], [st])
                RCP(st[:, 12:16], st[:, 12:16], [st], [st])
                TT(pcbv[:, :, 0:ncv], ecv[:, :, 0:ncv], blast(st[:, 12:16], ncv), ALU.mult, [ec, st], [pcb])
                yield
                for g in range(4):
                    TR(PBb[7][0:ncv, g * 128:(g + 1) * 128], pcbv[:, g, 0:ncv], identb, [pcb, cb], [PB[7]])
                CPA(pctv[0:ncv, :, :], PBb[7][0:ncv, 0:512].rearrange("p (g n) -> p g n", g=4), [PB[7]], [pct])
                yield
                po = PB[6]
                for g in range(4):
                    MM(po[:, 256:288], pctv[0:ncv, g, :], cb[0:ncv, B_OV:B_OV + 32], g == 0, g == 3,
                       [pct, cb], [po], skip=True)
                for g in range(4):
                    MM(po[:, g * 64:(g + 1) * 64], pctv[0:ncv, g, :], vcmpv[0:ncv, hk, :], False, True,
                       [pct, vcmp], [po], skip=True)
                TT(sc.ap, po[:, 256:288], cfa[:, C_V01 + i * 32:C_V01 + (i + 1) * 32], ALU.mult, [po, cfa], [sc])
                TT(sc.ap, sc.ap, cfa[:, C_FB2 + i * 32:C_FB2 + (i + 1) * 32], ALU.add, [sc, cfa], [sc])
                S.op("dve", lambda e, o=st[:, 16:24], a=sc.ap: e.max(out=o, in_=a), [sc.r], [st.r])
                TS(sc.ap, sc.ap, st[:, 23:24], 30000.0, ALU.is_ge, ALU.mult, [sc, st], [sc])
                TS(nsl.ap, sc.ap, -30000.0, None, ALU.add, None, [sc], [nsl])
                ocv = oc_.ap.rearrange("p (g d) -> p g d", g=4)
                TT(ocv, po[:, 0:256].rearrange("p (g d) -> p g d", g=4), blast(sg_[:, hk * 4:hk * 4 + 4], 64),
                   ALU.mult, [po, sg_], [oc_])
                yield
                TR(PBb[7][0:32, 0:128], nsl.ap, identb, [nsl, cb], [PB[7]])
                CPA(rbv_[0:32, :, :], bmid(PBb[7][0:32, 0:128], 4), [PB[7]], [rb_])

            ptc = [0]

            def att_back(n, gen_next):
                i, hk = iters[n]
                p = n % 2
                tsl = slice(i * 128, (i + 1) * 128)
                qt2_, rb_, oc_, sg_ = qt2s[p], rbs[p], ocs[p], sgs[i % 2]
                pos_ = PB[4]
                pow_ = PB[5]
                jlo = max(0, i - 4)
                items = [("s", j) for j in range(i + 1)] + [("w", j) for j in range(jlo, i + 1)]

                def emit_S(m, base):
                    kind, j = items[m]
                    ps = PB[2 + ((base + m) % 2)]
                    if kind == "s":
                        MM(ps.ap, kswv[0:64, hk, j * 128:(j + 1) * 128], qt2_[0:64, :], True, False, [ksw, qt2_], [ps])
                        MM(ps.ap, Ev[:, j, :], rb_[0:32, :], False, j != i, [cb, rb_], [ps])
                        if j == i:
                            MM(ps.ap, identb, cb[:, B_CAUS:B_CAUS + 512], False, True, [cb], [ps])
                    else:
                        msk = None
                        if j == i:
                            msk = B_CAUS
                        elif j == i - 4:
                            msk = B_ANTI
                        MM(ps.ap, kswv[64:128, hk, j * 128:(j + 1) * 128], qt2_[64:128, :], True, msk is None,
                           [ksw, qt2_], [ps])
                        if msk is not None:
                            MM(ps.ap, identb, cb[:, msk:msk + 512], False, True, [cb], [ps])

                def emit_EXP_PV(m, base):
                    kind, j = items[m]
                    ps = PB[2 + ((base + m) % 2)]
                    ptb = pt[(base + m) % 2]
                    ACT(ptb.ap, ps.ap, AF.Exp, [ps], [ptb], scale=0.125)
                    if kind == "s":
                        for g in range(4):
                            MM(pos_[:, g * 65:(g + 1) * 65], ptb[:, g * 128:(g + 1) * 128], vswv[:, j, hk, 0, :],
                               (j == 0 and g == 0), j == i, [ptb, vsw], [pos_], skip=True)
                    else:
                        for g in range(4):
                            MM(pow_[:, g * 65:(g + 1) * 65], ptb[:, g * 128:(g + 1) * 128], vswv[:, j, hk, 1, :],
                               (j == jlo and g == 0), j == i, [ptb, vsw], [pow_], skip=True)

                base = ptc[0]
                stride = max(1, len(items) // 6)
                emit_S(0, base)
                for m in range(len(items)):
                    if m + 1 < len(items):
                        emit_S(m + 1, base)
                    emit_EXP_PV(m, base)
                    if gen_next is not None and m % stride == stride - 1:
                        next(gen_next, None)
                ptc[0] += len(items)
                if gen_next is not None:
                    for _ in gen_next:
                        pass
                accv = acc.ap.rearrange("p (g d) -> p g d", g=4)
                tmpv = tmp.ap.rearrange("p (g d) -> p g d", g=4)
                for br, pacc in ((1, pos_), (2, pow_)):
                    pv = pacc[:, 0:260].rearrange("p (g d) -> p g d", g=4)
                    TS(fac[:, 0:4], pv[:, :, 64], 1e-30, None, ALU.max, None, [pacc], [fac])
                    RCP(fac[:, 0:4], fac[:, 0:4], [fac], [fac])
                    TT(fac[:, 4:8], fac[:, 0:4], sg_[:, br * 16 + hk * 4:br * 16 + hk * 4 + 4], ALU.mult,
                       [fac, sg_], [fac])
                    TT(tmpv, pv[:, :, 0:64], blast(fac[:, 4:8], 64), ALU.mult, [pacc, fac], [tmp])
                    if br == 1:
                        TT(acc.ap, tmp.ap, oc_.ap, ALU.add, [tmp, oc_], [acc])
                    else:
                        TT(btile.ap, tmp.ap, acc.ap, ALU.add, [tmp, acc], [btile])
                for hh in range(2):
                    TR(PBb[7][:, hh * 128:(hh + 1) * 128], btile[:, hh * 128:(hh + 1) * 128], identb,
                       [btile, cb], [PB[7]])
                CPV(bTv[:, hk * 2:hk * 2 + 2, tsl], PBb[7][:, 0:256].rearrange("p (c n) -> p c n", c=2),
                    [PB[7]], [bufA])

            if iters:
                for _ in att_front(0):
                    pass
                for n in range(len(iters)):
                    att_back(n, att_front(n + 1) if n + 1 < len(iters) else None)

            if stop < 3:
                continue
            S.barrier()
            cu = Cur(O_SCR)
            wh = [cu.take(8 * 512, BF16, f"wh{k}") for k in range(4)]

            def load_wh(h):
                b = wh[h % 4]
                DMA("pool", b.ap.rearrange("p (c n) -> p c n", c=8), whg[h], [], [b], b)

            def f32t(nm):
                return [cu.take(128, F32, f"{nm}{k}") for k in range(2)]

            def b16t(nm):
                return [cu.take(128, BF16, f"{nm}{k}") for k in range(2)]

            e1, kk, logf, eb, gs, sq = f32t("e1"), f32t("kk"), f32t("logf"), f32t("eb"), f32t("gs"), f32t("sq")
            qd, ki, ke, vv, attn, qd0, qd1, kit, ab = (b16t("qd"), b16t("ki"), b16t("ke"), b16t("vv"),
                                                       b16t("attn"), b16t("qd0"), b16t("qd1"), b16t("kit"), b16t("ab"))
            dec = f32t("dec")
            lhi, llo = b16t("lhi"), b16t("llo")
            sst = f32t("sst")
            Sf = f32t("Sf")
            Sb0, Sb1 = b16t("Sb0"), b16t("Sb1")
            for k in range(2):
                MSET(qd0[k].ap, 0.0, [qd0[k]])
                MSET(qd1[k].ap, 0.0, [qd1[k]])
            load_wh(0)
            load_wh(1)
            tri = cfa[:, C_TRI:C_TRI + 128]
            trev = cfa[:, C_TREV:C_TREV + 128]
            for pair in range(npair):
                if pair < 3:
                    load_wh(2 * pair + 2)
                    load_wh(2 * pair + 3)
                for k in range(2):
                    MSET(Sf[k].ap, 0.0, [Sf[k]])
                    MSET(Sb0[k].ap, 0.0, [Sb0[k]])
                def hg_proj(i_, k_):
                    h_ = 2 * pair + k_
                    whv_ = wh[h_ % 4].ap.rearrange("p (c n) -> p c n", c=8)
                    for c in range(8):
                        MM(PB[k_].ap, xTv[:, c, i_ * 128:(i_ + 1) * 128], whv_[:, c, :], c == 0, c == 7,
                           [xT, wh[h_ % 4]], [PB[k_]])

                hg_proj(0, 0)
                for i in range(ntile):
                    tsl = slice(i * 128, (i + 1) * 128)
                    for k in range(2):
                        h = 2 * pair + k
                        pp = PB[k]
                        if k == 0:
                            hg_proj(i, 1)
                        elif i + 1 < ntile:
                            hg_proj(i + 1, 0)
                        if hgcut < 1:
                            continue
                        rm = roml[:, h * 128:(h + 1) * 128]
                        ACT(e1[k].ap, pp[:, 128:256], AF.Exp, [pp], [e1[k]])
                        STT(e1[k].ap, e1[k].ap, 1.0, rm, ALU.add, ALU.mult, [e1[k], roml], [e1[k]])
                        RCP(kk[k].ap, e1[k].ap, [e1[k]], [kk[k]])
                        if hgcut < 2:
                            continue
                        ACT(logf[k].ap, kk[k].ap, AF.Ln, [kk[k], cfa], [logf[k]], scale=-1.0, bias=cfa[:, C_ONE:C_ONE + 1])
                        if hgcut < 3:
                            continue
                        pc_ = PB[2 + k]
                        CPA(lhi[k].ap, logf[k].ap, [logf[k]], [lhi[k]])
                        TT(llo[k].ap, logf[k].ap, lhi[k].ap, ALU.subtract, [logf[k], lhi[k]], [llo[k]])
                        if hgcut < 4:
                            continue
                        trib = cb[:, B_TRI:B_TRI + 128]
                        trevb = cb[:, B_TREV:B_TREV + 128]
                        cib = cb[:, B_CI:B_CI + 2]
                        MM(pc_[:, 0:128], trib, lhi[k].ap, True, False, [cb, lhi[k]], [pc_])
                        MM(pc_[:, 0:128], trib, llo[k].ap, False, True, [cb, llo[k]], [pc_])
                        MM(pc_[:, 128:256], trevb, lhi[k].ap, True, False, [cb, lhi[k]], [pc_])
                        MM(pc_[:, 128:256], trevb, llo[k].ap, False, True, [cb, llo[k]], [pc_])
                        if hgcut < 5:
                            continue
                        MM(pc_[:, 256:258], lhi[k].ap, cib, True, False, [cb, lhi[k]], [pc_])
                        MM(pc_[:, 256:258], llo[k].ap, cib, False, True, [cb, llo[k]], [pc_])
                        if hgcut < 6:
                            continue
                        ACT(eb[k].ap, pc_[:, 0:128], AF.Exp, [pc_], [eb[k]])
                        TT(qd[k].ap, pp[:, 0:128], eb[k].ap, ALU.mult, [pp, eb[k]], [qd[k]])
                        ACT(eb[k].ap, pc_[:, 0:128], AF.Exp, [pc_, qd[k]], [eb[k]], scale=-1.0)
                        TT(ki[k].ap, kk[k].ap, eb[k].ap, ALU.mult, [kk[k], eb[k]], [ki[k]])
                        ACT(eb[k].ap, pc_[:, 128:256], AF.Exp, [pc_, ki[k]], [eb[k]])
                        TT(ke[k].ap, kk[k].ap, eb[k].ap, ALU.mult, [kk[k], eb[k]], [ke[k]])
                        if hgcut < 7:
                            continue
                        ACT(dec[k][:, 0:2], pc_[:, 256:258], AF.Exp, [pc_], [dec[k]])
                        CPA(vv[k].ap, pp[:, 256:384], [pp], [vv[k]])
                        if hgcut < 8:
                            continue
                        ACT(gs[k].ap, pp[:, 384:512], AF.Exp, [pp], [gs[k]], scale=-1.0)
                        TS(gs[k].ap, gs[k].ap, 1.0, None, ALU.add, None, [gs[k]], [gs[k]])
                        RCP(gs[k].ap, gs[k].ap, [gs[k]], [gs[k]])
                        TT(gs[k].ap, gs[k].ap, cfa[:, C_NG:C_NG + 128], ALU.mult, [gs[k], cfa], [gs[k]])
                        TT(gs[k].ap, pp[:, 384:512], gs[k].ap, ALU.mult, [gs[k], pp], [gs[k]])
                        if hgcut < 9:
                            continue
                        pt_ = PB[7]
                        TR(PBb[7][:, 0:128], qd[k].ap, identb, [qd[k], cb], [pt_])
                        TR(PBb[7][:, 128:256], ki[k].ap, identb, [ki[k], cb], [pt_])
                        CPA(qd0[k][:, 0:64], PBb[7][:, 0:64], [pt_], [qd0[k]])
                        CPV(qd1[k][:, 64:128], PBb[7][:, 64:128], [pt_], [qd1[k]])
                        CPA(kit[k].ap, PBb[7][:, 128:256], [pt_], [kit[k]])
                        if hgcut < 10:
                            continue
                        pa = PB[4 + k]
                        MM(pa[:, 0:128], kit[k].ap, qd0[k].ap, True, False, [kit[k], qd0[k]], [pa])
                        MM(pa[:, 0:128], kit[k].ap, qd1[k].ap, False, True, [kit[k], qd1[k]], [pa])
                        TT(attn[k].ap, pa[:, 0:128], tri, ALU.mult, [pa, cfa], [attn[k]])
                        if hgcut < 11:
                            continue
                        pu = PB[6]
                        MM(pu[:, 0:128], ke[k][0:64, :], vv[k][0:64, :], True, True, [ke[k], vv[k]], [pu])
                        MM(pc_[:, 384:512], ke[k][64:128, :], vv[k][64:128, :], True, True, [ke[k], vv[k]], [pc_])
                        if hgcut < 11.2:
                            continue
                        TS(Sf[k].ap, Sf[k].ap, dec[k][:, 0:1], None, ALU.mult, None, [Sf[k], dec[k]], [Sf[k]])
                        TT(Sf[k].ap, pu[:, 0:128], Sf[k].ap, ALU.add, [Sf[k], pu], [Sf[k]])
                        CPA(Sb1[k].ap, Sf[k].ap, [Sf[k]], [Sb1[k]])
                        if hgcut < 11.4:
                            continue
                        MM(pa[:, 128:256], attn[k].ap, vv[k].ap, True, False, [attn[k], vv[k]], [pa])
                        MM(pa[:, 128:256], qd0[k].ap, Sb0[k].ap, False, False, [qd0[k], Sb0[k]], [pa])
                        MM(pa[:, 128:256], qd1[k].ap, Sb1[k].ap, False, True, [qd1[k], Sb1[k]], [pa])
                        if hgcut < 11.6:
                            continue
                        TS(Sf[k].ap, Sf[k].ap, dec[k][:, 1:2], None, ALU.mult, None, [Sf[k], dec[k]], [Sf[k]])
                        TT(Sf[k].ap, pc_[:, 384:512], Sf[k].ap, ALU.add, [Sf[k], pc_], [Sf[k]])
                        CPA(Sb0[k].ap, Sf[k].ap, [Sf[k]], [Sb0[k]])
                        if hgcut < 12:
                            continue
                        MSET(sst[k][:, 0:1], 0.0, [sst[k]])
                        ACT(sq[k].ap, pa[:, 128:256], AF.Square, [pa, sst[k]], [sq[k], sst[k]], accum=sst[k][:, 0:1])
                        if hgcut < 13:
                            continue
                        ACT(sst[k][:, 1:2], sst[k][:, 0:1], AF.Ln, [sst[k], cfa], [sst[k]], scale=1.0 / 128, bias=cfa[:, C_E6:C_E6 + 1])
                        ACT(sst[k][:, 2:3], sst[k][:, 1:2], AF.Exp, [sst[k]], [sst[k]], scale=-0.5)
                        if hgcut < 14:
                            continue
                        STT(ab[k].ap, pa[:, 128:256], sst[k][:, 2:3], gs[k].ap, ALU.mult, ALU.mult,
                            [pa, sst[k], gs[k]], [ab[k]])
                        TR(PBb[7][:, 256:384], ab[k].ap, identb, [ab[k], cb], [pt_])
                        CPV(aTv[:, h, tsl], PBb[7][:, 256:384], [pt_], [bufB])

            if dbg and s == 0:
                S.barrier()
                for nm, b in (("d_aT", bufB), ("d_bT", bufA)):
                    dtmp = at(O_XT, 8192, F32, "dtmp" + nm)
                    for q in range(2):
                        CPV(dtmp.ap, b[:, q * 8192:(q + 1) * 8192], [b], [dtmp])
                        DMA("sp", dbg_out[nm][:, q * 8192:(q + 1) * 8192], dtmp.ap, [dtmp], [], dtmp, store=True)

            if stop < 4:
                continue
            S.barrier()
            cu = Cur(O_XT)
            xtg = cu.take(8 * 512, BF16, "xtg")
            xtgv = xtg.ap.rearrange("p (c n) -> p c n", c=8)
            hx = [cu.take(1024, F32, f"hx{k}") for k in range(4)]
            h1t = cu.take(8 * 512, BF16, "h1t")
            h1tv = h1t.ap.rearrange("p (c n) -> p c n", c=8)
            mgt = cu.take(8 * 512, BF16, "mgt")
            mgtv = mgt.ap.rearrange("p (c n) -> p c n", c=8)
            actt = cu.take(22 * 512, BF16, "actt")
            acttv = actt.ap.rearrange("p (c n) -> p c n", c=22)
            ring = [cu.take(4096, BF16, f"ring{k}") for k in range(4)]
            sga = cu.take(512, F32, "sga")
            sgb = cu.take(512, F32, "sgb")
            m1 = cu.take(512, F32, "m1")
            m2 = cu.take(512, F32, "m2")
            hb = cu.take(1024, BF16, "hb")
            lnp = cu.take(4096, F32, "lnp")
            lst = cu.take(16, F32, "lst")
            junk = m1
            DMA("sp", lnp.ap, lnp_d, [], [lnp], lnp)
            rc = [0]

            def wload(src3d, ncol_total, c0, ncols, kdim=8):
                b = ring[rc[0] % 4]
                rc[0] += 1
                v = b.ap[:, 0:kdim * ncols].rearrange("p (c n) -> p c n", c=kdim)
                DMA("pool", v, src3d[:, :, c0:c0 + ncols], [], [b], b)
                return b, v

            def layer_norm(src, gcol, bcol, dst_f32, dst_buf):
                MSET(lst[:, 8:12], 0.0, [lst])
                ACT(junk[:, 0:512], src[:, 0:512], AF.Identity, [src, lst], [junk, lst], accum=lst[:, 8:9])
                ACT(junk[:, 0:512], src[:, 512:1024], AF.Identity, [src, lst], [junk, lst], accum=lst[:, 10:11])
                ACT(junk[:, 0:512], src[:, 0:512], AF.Square, [src, lst], [junk, lst], accum=lst[:, 9:10])
                ACT(junk[:, 0:512], src[:, 512:1024], AF.Square, [src, lst], [junk, lst], accum=lst[:, 11:12])
                TT(lst[:, 0:2], lst[:, 8:10], lst[:, 10:12], ALU.add, [lst], [lst])
                TS(lst[:, 2:4], lst[:, 0:2], 1.0 / 1024, None, ALU.mult, None, [lst], [lst])
                TT(lst[:, 4:5], lst[:, 2:3], lst[:, 2:3], ALU.mult, [lst], [lst])
                TT(lst[:, 5:6], lst[:, 3:4], lst[:, 4:5], ALU.subtract, [lst], [lst])
                ACT(lst[:, 6:7], lst[:, 5:6], AF.Ln, [lst, cfa], [lst], bias=cfa[:, C_E5:C_E5 + 1])
                ACT(lst[:, 7:8], lst[:, 6:7], AF.Exp, [lst], [lst], scale=-0.5)
                TS(src.ap, src.ap, lst[:, 2:3], lst[:, 7:8], ALU.subtract, ALU.mult, [src, lst], [src])
                TT(src.ap, src.ap, lnp[:, gcol:gcol + 1024], ALU.mult, [src, lnp], [src])
                TT(dst_f32, src.ap, lnp[:, bcol:bcol + 1024], ALU.add, [src, lnp], [dst_buf])

            for grp in range(4):
                g0 = grp * 512
                gsl = slice(g0, g0 + 512)
                for tt in range(4):
                    i = grp * 4 + tt
                    xb = hx[tt]
                    DMA("sp", xb.ap, x[tok0 + i * 128: tok0 + (i + 1) * 128, :], [], [xb], xb)
                    for c in range(8):
                        pb = PB[c // 4]
                        TR(pb[:, (c % 4) * 128:(c % 4 + 1) * 128], xb[:, c * 128:(c + 1) * 128], identf, [xb, cfa], [pb])
                    CPA(xtgv[:, 0:4, tt * 128:(tt + 1) * 128], PB[0].ap.rearrange("p (c n) -> p c n", c=4), [PB[0]], [xtg])
                    CPV(xtgv[:, 4:8, tt * 128:(tt + 1) * 128], PB[1].ap.rearrange("p (c n) -> p c n", c=4), [PB[1]], [xtg])
                for cb2 in range(2):
                    bga, vga = wload(wga, 1024, cb2 * 512, 512)
                    buh, vuh = wload(wuh, 1024, cb2 * 512, 512)
                    bgb, vgb = wload(wgb, 1024, cb2 * 512, 512)
                    bun, vun = wload(wun, 1024, cb2 * 512, 512)
                    for cc in range(4):
                        csl = slice(cc * 128, (cc + 1) * 128)
                        oc_ = cb2 * 4 + cc
                        qa, qb, qc, qd_ = (PB[2], PB[3], PB[4], PB[5]) if cc % 2 == 0 else (PB[0], PB[1], PB[6], PB[7])
                        for c in range(8):
                            MM(qa.ap, vga[:, c, csl], xtgv[:, c, :], c == 0, c == 7, [bga, xtg], [qa])
                        for c in range(8):
                            MM(qb.ap, vuh[:, c, csl], aTv[:, c, gsl], c == 0, c == 7, [buh, bufB], [qb])
                        for c in range(8):
                            MM(qc.ap, vgb[:, c, csl], xtgv[:, c, :], c == 0, c == 7, [bgb, xtg], [qc])
                        for c in range(8):
                            MM(qd_.ap, vun[:, c, csl], bTv[:, c, gsl], c == 0, c == 7, [bun, bufA], [qd_])
                        ACT(sga.ap, qa.ap, AF.Sigmoid, [qa], [sga])
                        ACT(sgb.ap, qc.ap, AF.Sigmoid, [qc], [sgb])
                        TT(m1.ap, qb.ap, sga.ap, ALU.mult, [qb, sga], [m1])
                        TT(m2.ap, qd_.ap, sgb.ap, ALU.mult, [qd_, sgb], [m2])
                        TT(mgtv[:, oc_, :], m1.ap, m2.ap, ALU.add, [m1, m2], [mgt])
                bo0, vo0 = wload(wo, 1024, 0, 512)
                bo1, vo1 = wload(wo, 1024, 512, 512)
                for tt in range(4):
                    tl = slice(tt * 128, (tt + 1) * 128)
                    for hf, (bo, vo) in enumerate(((bo0, vo0), (bo1, vo1))):
                        pb = PB[2 * tt + hf]
                        for c in range(8):
                            MM(pb.ap, mgtv[:, c, tl], vo[:, c, :], c == 0, c == 7, [mgt, bo], [pb])
                for tt in range(4):
                    tl = slice(tt * 128, (tt + 1) * 128)
                    xb = hx[tt]
                    for hf in range(2):
                        pb = PB[2 * tt + hf]
                        STT(xb[:, hf * 512:(hf + 1) * 512], xb[:, hf * 512:(hf + 1) * 512], ALPHA, pb.ap,
                            ALU.mult, ALU.add, [xb, pb], [xb])
                    layer_norm(xb, 0, 1024, xb.ap, xb)
                    if dbg and s == 0:
                        i = grp * 4 + tt
                        DMA("sp", dbg_out["d_h1"][i * 128:(i + 1) * 128, :], xb.ap, [xb], [], xb, store=True)
                    CPA(hb.ap, xb.ap, [xb], [hb])
                    for c in range(8):
                        TR(PBb[c // 4][:, (c % 4) * 128:(c % 4 + 1) * 128], hb[:, c * 128:(c + 1) * 128], identb,
                           [hb, cb], [PB[c // 4]])
                    CPA(h1tv[:, 0:4, tl], PBb[0][:, 0:512].rearrange("p (c n) -> p c n", c=4), [PB[0]], [h1t])
                    CPV(h1tv[:, 4:8, tl], PBb[1][:, 0:512].rearrange("p (c n) -> p c n", c=4), [PB[1]], [h1t])
                for fb in range(6):
                    ncol = 512 if fb < 5 else 256
                    bg, vg = wload(wfg, DFF, fb * 512, ncol)
                    bu, vu = wload(wfu, DFF, fb * 512, ncol)
                    for cc in range(ncol // 128):
                        csl = slice(cc * 128, (cc + 1) * 128)
                        fc = fb * 4 + cc
                        pg, pu_ = PB[2 + 2 * (fc % 2)], PB[3 + 2 * (fc % 2)]
                        for c in range(8):
                            MM(pg.ap, vg[:, c, csl], h1tv[:, c, :], c == 0, c == 7, [bg, h1t], [pg])
                        for c in range(8):
                            MM(pu_.ap, vu[:, c, csl], h1tv[:, c, :], c == 0, c == 7, [bu, h1t], [pu_])
                        ACT(sga.ap, pg.ap, AF.Silu, [pg], [sga])
                        TT(acttv[:, fc, :], sga.ap, pu_.ap, ALU.mult, [sga, pu_], [actt])
                for db in range(6):
                    nk = 4 if db < 5 else 2
                    b = ring[rc[0] % 4]
                    rc[0] += 1
                    v = b.ap[:, 0:nk * 1024].rearrange("p (k n) -> p k n", k=nk)
                    DMA("pool", v, wfd[:, db * 4:db * 4 + nk, :], [], [b], b)
                    for tt in range(4):
                        tl = slice(tt * 128, (tt + 1) * 128)
                        for kq_ in range(nk):
                            kidx = db * 4 + kq_
                            for hf in range(2):
                                pb = PB[tt * 2 + hf]
                                MM(pb.ap, acttv[:, kidx, tl], v[:, kq_, hf * 512:(hf + 1) * 512], kidx == 0, kidx == 21,
                                   [actt, b], [pb])
                for tt in range(4):
                    xb = hx[tt]
                    for hf in range(2):
                        pb = PB[tt * 2 + hf]
                        STT(xb[:, hf * 512:(hf + 1) * 512], xb[:, hf * 512:(hf + 1) * 512], ALPHA, pb.ap,
                            ALU.mult, ALU.add, [xb, pb], [xb])
                for tt in range(4):
                    i = grp * 4 + tt
                    xb = hx[tt]
                    layer_norm(xb, 2048, 3072, xb.ap, xb)
                    DMA("sp", y[tok0 + i * 128: tok0 + (i + 1) * 128, :], xb.ap, [xb], [], xb, store=True)

        S.finish()
        S.emit()
    return nc


def _pc(M):
    n = M.shape[1]
    return np.ascontiguousarray(M.reshape(M.shape[0] // 128, 128, n).transpose(1, 0, 2))


def prep_common(inp):
    f32 = np.float32
    W = np.asarray(inp["w_in"], f32)[0]
    o = np.cumsum([0, 1024, 1024, 1024, 1024, 1024, 256, 256, 256, 256, 256, 256, 48, 1024, 1024])
    hq, hf, hi, hg, nq, kc, vc, ks, vs, kw, vw, ng, ga, gb = [int(v) for v in o[:14]]
    d = {}
    d["whg"] = np.stack([_pc(np.concatenate([W[:, hq + h * 128: hq + (h + 1) * 128], W[:, hf + h * 128: hf + (h + 1) * 128],
                                             W[:, hi + h * 128: hi + (h + 1) * 128], W[:, hg + h * 128: hg + (h + 1) * 128]], 1))
                         for h in range(8)])
    cols = []
    for k in range(4):
        sl = lambda b: W[:, b + k * 64: b + (k + 1) * 64]
        cols += [sl(ks), sl(kw), sl(kc), sl(vc), sl(vs), sl(vw)]
    d["wkv"] = _pc(np.concatenate(cols, 1))
    d["wq"] = _pc(W[:, nq:nq + 1024])
    d["wng"] = _pc(W[:, ng:ng + 48])
    d["wga"] = _pc(W[:, ga:ga + 1024])
    d["wgb"] = _pc(W[:, gb:gb + 1024])
    d["wuh"] = _pc(np.asarray(inp["w_up_hg"], f32)[0])
    d["wun"] = _pc(np.asarray(inp["w_up_nsa"], f32)[0])
    d["wo"] = _pc(np.asarray(inp["w_o"], f32)[0])
    d["wfg"] = _pc(np.asarray(inp["w_ffn_gate"], f32)[0])
    d["wfu"] = _pc(np.asarray(inp["w_ffn_up"], f32)[0])
    d["wfd"] = _pc(np.asarray(inp["w_ffn_down"], f32)[0])
    w1k = np.asarray(inp["cmp_k_w1"], f32)[0].reshape(32, 64, 256).transpose(1, 0, 2)
    w1v = np.asarray(inp["cmp_v_w1"], f32)[0].reshape(32, 64, 256).transpose(1, 0, 2)
    d["w1"] = np.ascontiguousarray(np.concatenate([w1k, w1v], 0))
    d["w2k"] = np.ascontiguousarray(np.asarray(inp["cmp_k_w2"], f32)[0].reshape(2, 128, 64).transpose(1, 0, 2))
    d["w2v"] = np.ascontiguousarray(np.asarray(inp["cmp_v_w2"], f32)[0].reshape(2, 128, 64).transpose(1, 0, 2))
    cfa = np.zeros((128, NCFA), f32)
    cfa[:, C_ID:C_ID + 128] = np.eye(128, dtype=f32)
    sidx = np.arange(128)[:, None]
    tidx = np.arange(128)[None, :]
    same = (sidx // 64) == (tidx // 64)
    cfa[:, C_TRI:C_TRI + 128] = (same & (sidx <= tidx)).astype(f32)
    cfa[:, C_TREV:C_TREV + 128] = (same & (sidx > tidx)).astype(f32)
    cfa[:, C_CI] = (np.arange(128) < 64)
    cfa[:, C_CI + 1] = (np.arange(128) >= 64)
    p = np.arange(128)[:, None]
    j = np.arange(8)[None, :]
    cfa[:, C_M8:C_M8 + 8] = np.where(p >= 16 * j + 15, 0.0, -1e30)
    cfa[:, C_INVF:C_INVF + 32] = (10000.0 ** (-np.arange(32, dtype=f32) / 32)).astype(f32)[None, :]
    cfa[:, C_NG:C_NG + 128] = np.asarray(inp["hg_norm_g"], f32)[0][None, :]
    b1k = np.asarray(inp["cmp_k_b1"], f32)[0]
    b1v = np.asarray(inp["cmp_v_b1"], f32)[0]
    cfa[:, C_B1 + 0] = b1k[0:128]
    cfa[:, C_B1 + 1] = b1k[128:256]
    cfa[:, C_B1 + 2] = b1v[0:128]
    cfa[:, C_B1 + 3] = b1v[128:256]
    cfa[0:64, C_B2K] = np.asarray(inp["cmp_k_b2"], f32)[0]
    v01 = np.zeros((128, 16, 32), f32)
    fb2 = np.zeros((128, 16, 32), f32)
    blk = np.arange(32)[None, :]
    for i in range(16):
        cur = (2 * i + (np.arange(128) >= 64))[:, None]
        valid = blk <= cur
        forced = (blk == 0) | (blk == cur) | (blk == cur - 1)
        v01[:, i, :] = valid
        fb2[:, i, :] = np.where(valid, 1000.0 * forced, -1.0)
    cfa[:, C_V01:C_V01 + 512] = v01.reshape(128, 512)
    cfa[:, C_FB2:C_FB2 + 512] = fb2.reshape(128, 512)
    cfa[:, C_E6] = 1e-6
    cfa[:, C_E5] = 1e-5
    cfa[:, C_ONE] = 1.0
    d["cfa"] = cfa
    cb = np.zeros((128, NCB), f32)
    cb[:, B_ID:B_ID + 128] = np.eye(128, dtype=f32)
    cs = np.arange(127)[:, None] * 16
    bs = np.arange(32)[None, :] * 64
    ov = np.clip(np.minimum(cs + 32, bs + 64) - np.maximum(cs, bs), 0, None) / 32.0
    cb[0:127, B_OV:B_OV + 32] = ov
    E = np.zeros((32, 16, 128), f32)
    for jj in range(16):
        for key in range(128):
            E[2 * jj + key // 64, jj, key] = 1.0
    cb[0:32, B_E:B_E + 2048] = E.reshape(32, 2048)
    kq = np.arange(128)
    caus = np.where(kq[:, None] > kq[None, :], -30000.0, 0.0).astype(f32)
    anti = np.where(kq[:, None] <= kq[None, :], -30000.0, 0.0).astype(f32)
    cb[:, B_CAUS:B_CAUS + 512] = np.tile(caus, (1, 4))
    cb[:, B_ANTI:B_ANTI + 512] = np.tile(anti, (1, 4))
    cb[:, B_ONES:B_ONES + 128] = 1.0
    cb[0:64, B_POST:B_POST + 32] = np.asarray(inp["cmp_k_pos"], f32)[0].T
    cb[64:128, B_POST:B_POST + 32] = np.asarray(inp["cmp_v_pos"], f32)[0].T
    cb[:, B_B2V:B_B2V + 64] = np.asarray(inp["cmp_v_b2"], f32)[0][None, :]
    cb[:, B_TRI:B_TRI + 128] = cfa[:, C_TRI:C_TRI + 128]
    cb[:, B_TREV:B_TREV + 128] = cfa[:, C_TREV:C_TREV + 128]
    cb[:, B_CI:B_CI + 2] = cfa[:, C_CI:C_CI + 2]
    d["cb"] = cb
    lb = np.asarray(inp["hg_lb_logits"], f32)
    d["lbrows"] = np.ascontiguousarray(np.broadcast_to(lb.reshape(1, 2048), (128, 2048)))
    lnp = np.concatenate([np.asarray(inp[k], f32)[0] for k in ("ln1_g", "ln1_b", "ln2_g", "ln2_b")])
    d["lnp"] = np.ascontiguousarray(np.broadcast_to(lnp[None, :], (128, 4096)))
    return d


_NC_CACHE = {}


def kernel(**inp):
    x = np.asarray(inp["x"], np.float32)
    pos = np.asarray(inp["positions"], np.int32)
    common = prep_common(inp)
    nseq = x.shape[0] // NCORES
    if nseq not in _NC_CACHE:
        _NC_CACHE[nseq] = build(nseq)
    nc = _NC_CACHE[nseq]
    in_maps = []
    for c in range(NCORES):
        m = dict(common)
        m["x"] = np.ascontiguousarray(x[c * nseq:(c + 1) * nseq].reshape(nseq * SEQ, DM))
        m["pos"] = np.ascontiguousarray(pos[c * nseq:(c + 1) * nseq].reshape(nseq, NT, 128).transpose(0, 2, 1))
        in_maps.append(m)
    res = run_bass_kernel_spmd(nc, in_maps, core_ids=list(range(NCORES)))
    out = np.concatenate([r["y"].reshape(nseq, SEQ, DM) for r in res.results], axis=0)
    return out.astype(np.float32)
```
